# Optimizing a Trainium2 kernel written in Bass

```python
import math
import jax, jax.numpy as jnp
from jax import lax
import numpy as np

D_MODEL = 1024
BATCH = 8
SEQ = 4096
DEPTH = 1

D_MIX = D_MODEL
DN_WIDTH = D_MIX // 2
DN_HEADS = 4
DN_HEAD_DIM = DN_WIDTH // DN_HEADS
CONV_K = 4
DN_CHUNK = 64
SGU_WIDTH = D_MIX - DN_WIDTH
SGU_GROUPS = 4
SGU_GROUP_DIM = SGU_WIDTH // SGU_GROUPS
SGU_CHUNK = 128
IN_COLS = 4 * DN_WIDTH + 2 * DN_HEADS + 2 * SGU_WIDTH
MOE_GROUPS = 8
EXPERTS_PER_GROUP = 8
N_EXPERTS = MOE_GROUPS * EXPERTS_PER_GROUP
TOP_K = 2
D_EXPERT = D_MODEL // 2
MOE_BLOCK = 128
DEEPNORM_ALPHA = (2.0 * DEPTH) ** 0.25
DEEPNORM_BETA = (8.0 * DEPTH) ** -0.25
LN_EPS = 1e-5
RMS_EPS = 1e-6

kernel_name = "hybrid_deltanet_sgu_hmoe_deepnorm"


def _layernorm(x, g, b):
    xf = x.astype(jnp.float32)
    mu = jnp.mean(xf, axis=-1, keepdims=True)
    xc = xf - mu
    var = jnp.mean(xc * xc, axis=-1, keepdims=True)
    return (xc * lax.rsqrt(var + LN_EPS) * g + b).astype(x.dtype)


def _l2norm(x):
    return x * lax.rsqrt(jnp.sum(x * x, axis=-1, keepdims=True) + RMS_EPS)


def _causal_conv_silu(x, w):
    y = lax.conv_general_dilated(
        x, w[:, None, :], window_strides=(1,), padding=[(CONV_K - 1, 0)],
        dimension_numbers=("NWC", "WIO", "NWC"), feature_group_count=x.shape[-1])
    return jax.nn.silu(y)


def _chunked_gated_delta_rule(q, k, v, g, beta):
    B, T, H, dk = q.shape
    dv = v.shape[-1]
    C = DN_CHUNK
    NC = T // C

    def to_chunks(t):
        return t.reshape(B, NC, C, H, t.shape[-1]).transpose(0, 3, 1, 2, 4)

    q, k, v = to_chunks(q), to_chunks(k), to_chunks(v)
    g = g.reshape(B, NC, C, H).transpose(0, 3, 1, 2)
    beta = beta.reshape(B, NC, C, H).transpose(0, 3, 1, 2)
    gc = jnp.cumsum(g, axis=-1)

    pos = jnp.arange(C)
    causal = pos[:, None] >= pos[None, :]
    strict = pos[:, None] > pos[None, :]
    decay = jnp.exp(jnp.where(causal, gc[..., :, None] - gc[..., None, :], -jnp.inf))

    kb = k * beta[..., None]
    kkt = jnp.einsum("bhnid,bhnjd->bhnij", kb, k) * decay
    a_mat = jnp.eye(C, dtype=jnp.float32) + jnp.where(strict, kkt, 0.0)
    rhs = jnp.concatenate([v * beta[..., None], kb * jnp.exp(gc)[..., None]], axis=-1)
    sol = lax.linalg.triangular_solve(a_mat, rhs, left_side=True, lower=True, unit_diagonal=True)
    u_vals, w_dec = sol[..., :dv], sol[..., dv:]

    qk_intra = jnp.einsum("bhnid,bhnjd->bhnij", q, k) * decay
    q_dec = q * jnp.exp(gc)[..., None]
    k_dec = k * jnp.exp(gc[..., -1:] - gc)[..., None]
    g_last = jnp.exp(gc[..., -1])

    xs = tuple(jnp.moveaxis(t, 2, 0) for t in (q_dec, k_dec, u_vals, w_dec, qk_intra, g_last))

    def step(S, inp):
        qd, kd, u, w, a_intra, gl = inp
        v_new = u - jnp.einsum("bhck,bhkv->bhcv", w, S)
        o = jnp.einsum("bhck,bhkv->bhcv", qd, S) + jnp.einsum("bhij,bhjv->bhiv", a_intra, v_new)
        S = S * gl[..., None, None] + jnp.einsum("bhck,bhcv->bhkv", kd, v_new)
        return S, o

    S0 = jnp.zeros((B, H, dk, dv), jnp.float32)
    _, o = lax.scan(step, S0, xs)
    return o.transpose(1, 0, 3, 2, 4).reshape(B, T, H, dv)


def _gated_deltanet(qkv, z, b_raw, a_raw, conv_w, a_log, dt_bias, norm_w):
    B, T, _ = qkv.shape
    qkv = _causal_conv_silu(qkv.astype(jnp.float32), conv_w.astype(jnp.float32))
    q, k, v = jnp.split(qkv, 3, axis=-1)
    q = _l2norm(q.reshape(B, T, DN_HEADS, DN_HEAD_DIM)) * (DN_HEAD_DIM ** -0.5)
    k = _l2norm(k.reshape(B, T, DN_HEADS, DN_HEAD_DIM))
    v = v.reshape(B, T, DN_HEADS, DN_HEAD_DIM)
    beta = jax.nn.sigmoid(b_raw.astype(jnp.float32))
    g = -jnp.exp(a_log.astype(jnp.float32)) * jax.nn.softplus(a_raw.astype(jnp.float32) + dt_bias.astype(jnp.float32))
    o = _chunked_gated_delta_rule(q, k, v, g, beta)
    zf = z.astype(jnp.float32).reshape(B, T, DN_HEADS, DN_HEAD_DIM)
    o = o * lax.rsqrt(jnp.mean(o * o, axis=-1, keepdims=True) + RMS_EPS) * norm_w * jax.nn.silu(zf)
    return o.reshape(B, T, DN_WIDTH)


def _spatial_gating(u, v, ln_g, ln_b, w_spatial, b_spatial):
    B, T, _ = u.shape
    n_chunks = T // SGU_CHUNK
    u = jax.nn.gelu(u)
    v = jax.nn.gelu(v).reshape(B, n_chunks, SGU_CHUNK, SGU_GROUPS, SGU_GROUP_DIM)
    v = _layernorm(v, ln_g.reshape(SGU_GROUPS, SGU_GROUP_DIM), ln_b.reshape(SGU_GROUPS, SGU_GROUP_DIM))
    mask = jnp.tril(jnp.ones((SGU_CHUNK, SGU_CHUNK), dtype=bool))
    ws = jnp.where(mask, w_spatial, 0.0).astype(v.dtype)
    mixed = jnp.einsum("gts,bnsgd->bntgd", ws, v) + b_spatial.T[None, None, :, :, None].astype(v.dtype)
    return u * mixed.reshape(B, T, SGU_WIDTH)


def _mixer(h, w_in, conv_w, a_log, dt_bias, dn_norm_w, sgu_ln_g, sgu_ln_b, w_spatial, b_spatial, w_out):
    proj = h @ w_in
    splits = [3 * DN_WIDTH, 4 * DN_WIDTH, 4 * DN_WIDTH + DN_HEADS,
              4 * DN_WIDTH + 2 * DN_HEADS, 4 * DN_WIDTH + 2 * DN_HEADS + SGU_WIDTH]
    qkv, z, b_raw, a_raw, u, v = jnp.split(proj, splits, axis=-1)
    y_dn = _gated_deltanet(qkv, z, b_raw, a_raw, conv_w, a_log, dt_bias, dn_norm_w).astype(h.dtype)
    y_sgu = _spatial_gating(u, v, sgu_ln_g, sgu_ln_b, w_spatial, b_spatial)
    return jnp.concatenate([y_dn, y_sgu], axis=-1) @ w_out


def _hierarchical_moe(h, w_rg, b_rg, w_re, b_re, w_gate, w_up, w_down):
    B, T, D = h.shape
    xf = h.reshape(B * T, D)
    N = xf.shape[0]
    group_logits = (xf @ w_rg).astype(jnp.float32) + b_rg.astype(jnp.float32)
    g_idx = jnp.argmax(group_logits, axis=-1)
    p_group = jnp.take_along_axis(jax.nn.softmax(group_logits, axis=-1), g_idx[:, None], axis=-1)
    exp_logits = ((xf @ w_re).astype(jnp.float32) + b_re.astype(jnp.float32)).reshape(N, MOE_GROUPS, EXPERTS_PER_GROUP)
    within = jnp.take_along_axis(exp_logits, g_idx[:, None, None], axis=1)[:, 0]
    top_vals, top_idx = lax.top_k(within, TOP_K)
    weights = p_group * jax.nn.softmax(top_vals, axis=-1)
    expert_ids = g_idx[:, None] * EXPERTS_PER_GROUP + top_idx

    A = N * TOP_K
    e_flat = expert_ids.reshape(A).astype(jnp.int32)
    tok_flat = jnp.repeat(jnp.arange(N, dtype=jnp.int32), TOP_K)
    w_flat = weights.reshape(A)
    order = jnp.argsort(e_flat)
    e_sorted, tok_sorted, w_sorted = e_flat[order], tok_flat[order], w_flat[order]
    counts = jnp.zeros((N_EXPERTS,), jnp.int32).at[e_flat].add(1)
    padded = ((counts + MOE_BLOCK - 1) // MOE_BLOCK) * MOE_BLOCK
    starts = jnp.cumsum(counts) - counts
    pends = jnp.cumsum(padded)
    pstarts = pends - padded
    dest = pstarts[e_sorted] + (jnp.arange(A, dtype=jnp.int32) - starts[e_sorted])
    P = (-(-A // MOE_BLOCK)) * MOE_BLOCK + N_EXPERTS * MOE_BLOCK
    NB = P // MOE_BLOCK
    row_tok = jnp.full((P,), N, jnp.int32).at[dest].set(tok_sorted)
    row_w = jnp.zeros((P,), h.dtype).at[dest].set(w_sorted.astype(h.dtype))
    block_start = jnp.arange(NB, dtype=jnp.int32) * MOE_BLOCK
    block_e = jnp.minimum(jnp.searchsorted(pends, block_start, side="right"), N_EXPERTS - 1)

    x_pad = jnp.concatenate([xf, jnp.zeros((1, D), xf.dtype)], axis=0)
    xs = x_pad[row_tok].reshape(NB, MOE_BLOCK, D)

    def expert_block(args):
        xb, e = args
        hid = jax.nn.silu(xb @ w_gate[e]) * (xb @ w_up[e])
        return hid @ w_down[e]

    ys = lax.map(expert_block, (xs, block_e)).reshape(P, D) * row_w[:, None]
    out = jnp.zeros((N + 1, D), h.dtype).at[row_tok].add(ys)[:N]
    return out.reshape(B, T, D)


def setup_inputs(seed: int = 0) -> dict:
    key = jax.random.key(seed)
    ks = jax.random.split(key, 24)
    f32 = jnp.float32
    L = DEPTH

    def nrm(k, shape, scale):
        return jax.random.normal(k, shape, f32) * scale

    x = jax.random.normal(ks[0], (BATCH, SEQ, D_MODEL), f32)
    w_in = nrm(ks[1], (L, D_MODEL, IN_COLS), D_MODEL ** -0.5)
    conv_w = nrm(ks[2], (L, CONV_K, 3 * DN_WIDTH), CONV_K ** -0.5)
    a_log = jnp.log(jax.random.uniform(ks[3], (L, DN_HEADS), f32, 1.0, 16.0))
    dt = jnp.exp(jax.random.uniform(ks[4], (L, DN_HEADS), f32, math.log(1e-3), math.log(1e-1)))
    dt_bias = dt + jnp.log(-jnp.expm1(-dt))
    dn_norm_w = 1.0 + nrm(ks[5], (L, DN_HEAD_DIM), 0.02)
    sgu_ln_g = 1.0 + nrm(ks[6], (L, SGU_WIDTH), 0.02)
    sgu_ln_b = nrm(ks[7], (L, SGU_WIDTH), 0.02)
    tri = jnp.tril(jnp.ones((SGU_CHUNK, SGU_CHUNK), f32))
    w_spatial = nrm(ks[8], (L, SGU_GROUPS, SGU_CHUNK, SGU_CHUNK), SGU_CHUNK ** -0.5) * tri
    b_spatial = 1.0 + nrm(ks[9], (L, SGU_GROUPS, SGU_CHUNK), 0.02)
    w_out = nrm(ks[10], (L, D_MIX, D_MODEL), DEEPNORM_BETA * D_MIX ** -0.5)
    ln1_g = 1.0 + nrm(ks[11], (L, D_MODEL), 0.02)
    ln1_b = nrm(ks[12], (L, D_MODEL), 0.02)
    w_router_group = nrm(ks[13], (L, D_MODEL, MOE_GROUPS), D_MODEL ** -0.5)
    b_router_group = nrm(ks[14], (L, MOE_GROUPS), 0.01)
    w_router_expert = nrm(ks[15], (L, D_MODEL, N_EXPERTS), D_MODEL ** -0.5)
    b_router_expert = nrm(ks[16], (L, N_EXPERTS), 0.01)
    w_gate = nrm(ks[17], (L, N_EXPERTS, D_MODEL, D_EXPERT), D_MODEL ** -0.5)
    w_up = nrm(ks[18], (L, N_EXPERTS, D_MODEL, D_EXPERT), D_MODEL ** -0.5)
    w_down = nrm(ks[19], (L, N_EXPERTS, D_EXPERT, D_MODEL), DEEPNORM_BETA * D_EXPERT ** -0.5)
    ln2_g = 1.0 + nrm(ks[20], (L, D_MODEL), 0.02)
    ln2_b = nrm(ks[21], (L, D_MODEL), 0.02)
    return {"x": x, "w_in": w_in, "conv_w": conv_w, "a_log": a_log, "dt_bias": dt_bias,
            "dn_norm_w": dn_norm_w, "sgu_ln_g": sgu_ln_g, "sgu_ln_b": sgu_ln_b,
            "w_spatial": w_spatial, "b_spatial": b_spatial, "w_out": w_out,
            "ln1_g": ln1_g, "ln1_b": ln1_b,
            "w_router_group": w_router_group, "b_router_group": b_router_group,
            "w_router_expert": w_router_expert, "b_router_expert": b_router_expert,
            "w_gate": w_gate, "w_up": w_up, "w_down": w_down,
            "ln2_g": ln2_g, "ln2_b": ln2_b}


def reference(x, w_in, conv_w, a_log, dt_bias, dn_norm_w, sgu_ln_g, sgu_ln_b, w_spatial, b_spatial,
              w_out, ln1_g, ln1_b, w_router_group, b_router_group, w_router_expert, b_router_expert,
              w_gate, w_up, w_down, ln2_g, ln2_b):
    h = x
    for l in range(DEPTH):
        mix = _mixer(h, w_in[l], conv_w[l], a_log[l], dt_bias[l], dn_norm_w[l], sgu_ln_g[l], sgu_ln_b[l],
                     w_spatial[l], b_spatial[l], w_out[l])
        h = _layernorm(DEEPNORM_ALPHA * h + mix, ln1_g[l], ln1_b[l])
        ffn = _hierarchical_moe(h, w_router_group[l], b_router_group[l], w_router_expert[l], b_router_expert[l],
                                w_gate[l], w_up[l], w_down[l])
        h = _layernorm(DEEPNORM_ALPHA * h + ffn, ln2_g[l], ln2_b[l])
    return h
```

```python
import bisect
from contextlib import ExitStack

import numpy as np
import concourse.bass as bass
import concourse.mybir as mybir
from concourse.bass_utils import run_bass_kernel_spmd

F32 = mybir.dt.float32
BF16 = mybir.dt.bfloat16
I32 = mybir.dt.int32
AF = mybir.ActivationFunctionType
ALU = mybir.AluOpType
AX = mybir.AxisListType

SEQ = 4096
D = 1024
NT = SEQ // 128
IN_COLS = 3080
C_Z = 1536
C_SP = 2048
C_U = 2056
C_VS = 2568
NE = 64
CAP = 384
RT = CAP // 128
TRASH = NE * CAP
NROWS = NE * CAP + 128
ALPHA = 2.0 ** 0.25
LN_EPS = 1e-5
RMS_EPS = 1e-6
NEG = -30000.0
ZF = True
SAME_ENGINE_RAW = True
EAGER_INC = True
LIST_SCHED = True
BANK_SPLIT = (3, 4, 1)
STRICT_SAME_ENGINE = True
SAME_ENGINE_GAP = 1000000
TILE_STEPS_PER_BLOCK_STEP = 1
BLOCK_STEPS_PER_TILE_STEP = 2
TPB = 2
BW = TPB * 128
NBLK = NT // TPB

RP = {}
_o = 0
for _n, _w in [("a_log", 4), ("dt_bias", 4), ("sgu_g", 512), ("sgu_b", 512), ("bsp", 512),
               ("ln1_g", 1024), ("ln1_b", 1024),
               ("br", 72), ("iota", 64)]:
    RP[_n] = (_o, _w)
    _o += _w
NRP = _o
CS = {n: i for i, n in enumerate(["ident", "ones", "tri", "chs", "maskS", "maskST", "maskIT",
                                  "sltri", "maskWS"])}
NCS = len(CS)


import heapq
import types

MODE = {"m": "emit"}


def _free_elems(ap):
    sh = ap.shape
    n = 1
    for v in sh[1:]:
        n *= int(v)
    return n


LINT = None


def _lint_check(tag, r, w, keyf):
    rk = set(keyf(t) for t in r)
    wk = set(keyf(t) for t in w)
    for name, is_out in LINT:
        if name.startswith("psb"):
            tok = ("ps", int(name[3:]))
        elif name.startswith("sb_"):
            tok = name[3:]
        else:
            continue
        if is_out:
            if tok not in wk:
                print("LINT: %s writes %s without declaring it (w=%s)" % (tag, tok, sorted(map(str, wk))))
        elif tok not in rk and tok not in wk:
            print("LINT: %s reads %s without declaring it (r=%s w=%s)" % (tag, tok, sorted(map(str, rk)), sorted(map(str, wk))))
    del LINT[:]


class EngProxy:
    def __init__(self, real, kind):
        self._real = real
        self._kind = kind

    def __getattr__(self, name):
        real = getattr(self._real, name) if self._real is not None else None
        kind = self._kind

        def call(*a, **kw):
            if MODE["m"] == "emit":
                return real(*a, **kw)
            if LINT is not None:
                outs = [kw["out"]] if "out" in kw else list(a[:1])
                for v in list(a) + list(kw.values()):
                    ap_ = getattr(v, "ap", v) if v.__class__.__name__ == "IndirectOffsetOnAxis" else v
                    if hasattr(ap_, "tensor") and hasattr(ap_, "shape"):
                        LINT.append((str(getattr(ap_.tensor, "name", ap_.name)), any(v is o for o in outs)))
            out = kw.get("out", a[0] if a else None)
            n = _free_elems(out) if out is not None and hasattr(out, "shape") else 64
            if kind == "pe":
                src = a[1] if len(a) > 1 else kw.get("in_", kw.get("lhsT"))
                mult = 1.0
                try:
                    if src.dtype == F32:
                        mult = 4.0 if name == "matmul" else 2.0
                except Exception:
                    pass
                return 0.03 + n * 0.65e-3 * mult
            if kind == "act":
                return 0.2 + n * 0.95e-3
            if kind == "dve":
                return 0.12 + n * 1.05e-3
            if kind == "pool":
                return 0.15 + n * 1.9e-3
            tot = n * int(out.shape[0]) if out is not None and hasattr(out, "shape") else 1 << 16
            src = kw.get("in_", a[1] if len(a) > 1 else None)
            if src is not None and hasattr(src, "shape"):
                tot = min(tot, _free_elems(src) * int(src.shape[0]))
            return ("dma", 2.0 + tot * 3.0 / 150e3)
        return call


def _snapshot(fn):
    if fn.__closure__ is None:
        return fn
    cells = []
    for c in fn.__closure__:
        try:
            cells.append(types.CellType(c.cell_contents))
        except ValueError:
            cells.append(c)
    return types.FunctionType(fn.__code__, fn.__globals__, fn.__name__, fn.__defaults__, tuple(cells))


class Buf:
    def __init__(self, t, key):
        self.t = t
        self.k = key

    def __getitem__(self, idx):
        return self.t[idx]


class Sched:
    def __init__(self, nc, es):
        self.nc = nc
        self.es = es
        self.eng = {"pe": nc.tensor, "dve": nc.vector, "act": nc.scalar, "pool": nc.gpsimd,
                    "sp": nc.sync}
        self.sem = {k: es.enter_context(nc.semaphore("sem_" + k)) for k in ("pe", "dve", "act", "pool")}
        self.insts = {k: [] for k in self.sem}
        self.inc_idx = {k: [] for k in self.sem}
        self.inc_cnt = {k: [] for k in self.sem}
        self.seen = {k: {} for k in self.eng}
        self.dsem = {}
        self.last_w = {}
        self.readers = {}
        self.n_wait = 0
        self.log = None
        self.defer = LIST_SCHED
        self.rec = []
        self.rlast_w = {}
        self.rreaders = {}
        self.rkey_last = {}
        self.mq = EngProxy(None, "dmaq")
        self.pe_prev = None
        self.label = ""
        self.labels = {k: [] for k in self.sem}

    @staticmethod
    def _key(b):
        return b.k if isinstance(b, Buf) else b

    def _resolve(self, ref):
        if ref[0] == "e":
            _, eng, idx = ref
            ii = self.inc_idx[eng]
            p = bisect.bisect_left(ii, idx)
            if p < len(ii):
                return ("e", eng), self.sem[eng], self.inc_cnt[eng][p]
            cnt = (self.inc_cnt[eng][-1] if ii else 0) + 1
            self.insts[eng][idx].then_inc(self.sem[eng], 1)
            if self.log is not None:
                self.log.append("   inc %s[%d] -> %d" % (eng, idx, cnt))
            ii.append(idx)
            self.inc_cnt[eng].append(cnt)
            return ("e", eng), self.sem[eng], cnt
        _, key, cnt = ref
        return ("d", key), self.dsem[key][0], cnt

    def _wait(self, consumer, ref, raw=False):
        if ref[0] == "e" and ref[1] == consumer:
            if consumer == "pe" or not SAME_ENGINE_RAW:
                return
            if not raw and not STRICT_SAME_ENGINE:
                return
            if len(self.insts[consumer]) - ref[2] > SAME_ENGINE_GAP:
                return
        name, sem, cnt = self._resolve(ref)
        if self.seen[consumer].get(name, 0) >= cnt:
            return
        self.eng[consumer].wait_ge(sem, cnt)
        if self.log is not None:
            self.log.append("   %s waits %s >= %d" % (consumer, name, cnt))
        self.seen[consumer][name] = cnt
        self.n_wait += 1

    def _deps(self, consumer, reads, writes):
        for t in reads:
            k = self._key(t)
            if k in self.last_w:
                self._wait(consumer, self.last_w[k], raw=True)
        for t in writes:
            k = self._key(t)
            if k in self.last_w:
                self._wait(consumer, self.last_w[k])
            for r in self.readers.get(k, ()):
                self._wait(consumer, r)

    def _record(self, ref, reads, writes):
        for t in reads:
            k = self._key(t)
            lst = self.readers.setdefault(k, [])
            src = ref[:2]
            lst[:] = [r for r in lst if r[:2] != src]
            lst.append(ref)
        for t in writes:
            k = self._key(t)
            self.last_w[k] = ref
            self.readers[k] = []

    def _psx(self, r, w):
        ps = [t for t in r if isinstance(self._key(t), tuple) and self._key(t)[0] == "ps"]
        if not ps:
            return r, w
        return [t for t in r if t not in ps], list(w) + [t for t in ps if t not in w]

    def _rec_deps(self, r, w):
        deps = set()
        for t in r:
            k = self._key(t)
            if k in self.rlast_w:
                deps.add(self.rlast_w[k])
        for t in w:
            k = self._key(t)
            if k in self.rlast_w:
                deps.add(self.rlast_w[k])
            deps.update(self.rreaders.get(k, ()))
        return deps

    def _rec_note(self, i, r, w):
        for t in r:
            self.rreaders.setdefault(self._key(t), []).append(i)
        for t in w:
            k = self._key(t)
            self.rlast_w[k] = i
            self.rreaders[k] = []

    def op(self, eng, fn, r=(), w=()):
        if not self.defer:
            return self._emit_op(eng, fn, r, w)
        r2, w2 = self._psx(r, w)
        fn = _snapshot(fn)
        MODE["m"] = "measure"
        try:
            cost = fn()
        finally:
            MODE["m"] = "emit"
        if LINT is not None:
            _lint_check("op %s #%d [%s]" % (eng, len(self.rec), self.label), r, w, self._key)
        i = len(self.rec)
        deps = self._rec_deps(r2, w2)
        self.rec.append(("op", eng, fn, r, w, float(cost), deps, self.label))
        self._rec_note(i, r2, w2)

    def dma(self, q, key, fn, r=(), w=()):
        if not self.defer:
            return self._emit_dma(q, key, fn, r, w)
        fn = _snapshot(fn)
        MODE["m"] = "measure"
        try:
            cost = fn(self.mq)
        finally:
            MODE["m"] = "emit"
        lat = cost[1] if isinstance(cost, tuple) else 3.0
        if LINT is not None:
            _lint_check("dma %s key=%s" % (q, key), r, w, self._key)
        i = len(self.rec)
        deps = self._rec_deps(r, w)
        kdep = self.rkey_last.get(key, -1)
        self.rkey_last[key] = i
        self.rec.append(("dma", q, fn, r, w, lat, deps, key, kdep))
        self._rec_note(i, r, w)

    def flush(self):
        rec = self.rec
        n = len(rec)
        if n == 0:
            return
        succ = [[] for _ in range(n)]
        ksucc = [-1] * n
        ndep = [0] * n
        for i, e in enumerate(rec):
            ndep[i] = len(e[6])
            for d in e[6]:
                succ[d].append(i)
            if e[0] == "dma" and e[8] >= 0 and e[8] not in e[6]:
                ksucc[e[8]] = i
                ndep[i] += 1
        blev = [0.0] * n
        for i in range(n - 1, -1, -1):
            e = rec[i]
            b = 0.0
            for j in succ[i]:
                if blev[j] > b:
                    b = blev[j]
            blev[i] = b + e[5] + 0.3
        ready_t = [0.0] * n
        finish = [0.0] * n
        eng_free = {}
        engs = {}
        for i in range(n):
            engs.setdefault(rec[i][1], [[], []])
        for i in range(n):
            if ndep[i] == 0:
                heapq.heappush(engs[rec[i][1]][0], (0.0, i))
        order = []
        t_base = 0.0
        n_done = 0
        while n_done < n:
            best = None
            for eng, (fut, rdy) in engs.items():
                free = eng_free.get(eng, t_base)
                while fut and fut[0][0] <= free:
                    rt_, j = heapq.heappop(fut)
                    heapq.heappush(rdy, (-blev[j], j))
                if rdy:
                    cand = (free, eng)
                elif fut:
                    cand = (fut[0][0], eng)
                else:
                    continue
                if best is None or cand < best:
                    best = cand
            tstart, eng = best
            fut, rdy = engs[eng]
            if not rdy:
                while fut and fut[0][0] <= tstart:
                    rt_, j = heapq.heappop(fut)
                    heapq.heappush(rdy, (-blev[j], j))
            _, i = heapq.heappop(rdy)
            n_done += 1
            e = rec[i]
            est = ready_t[i]
            start = max(est, eng_free.get(eng, t_base))
            if e[0] == "op":
                fin = start + e[5]
                eng_free[eng] = fin
            else:
                issue = 0.15 if eng == "sp" else 0.8
                eng_free[eng] = start + issue
                fin = start + e[5]
            finish[i] = fin
            order.append(i)
            for j in succ[i]:
                lat = 0.05 if rec[j][1] == eng and e[0] == "op" else 0.3
                ready_t[j] = max(ready_t[j], fin + lat)
                ndep[j] -= 1
                if ndep[j] == 0:
                    heapq.heappush(engs[rec[j][1]][0], (ready_t[j], j))
            j = ksucc[i]
            if j >= 0:
                ready_t[j] = max(ready_t[j], eng_free[eng])
                ndep[j] -= 1
                if ndep[j] == 0:
                    heapq.heappush(engs[rec[j][1]][0], (ready_t[j], j))
        assert len(order) == n
        self.sim_time = getattr(self, "sim_time", 0.0) + max(finish)
        self.rec = []
        self.rlast_w = {}
        self.rreaders = {}
        self.rkey_last = {}
        for i in order:
            e = rec[i]
            if e[0] == "op":
                self.label = e[7]
                self._emit_op(e[1], e[2], e[3], e[4])
            else:
                self._emit_dma(e[1], e[7], e[2], e[3], e[4])

    def _emit_op(self, eng, fn, r=(), w=()):
        r, w = self._psx(r, w)
        self._deps(eng, r, w)
        inst = fn()
        idx = len(self.insts[eng])
        self.insts[eng].append(inst)
        self.labels[eng].append(self.label)
        if EAGER_INC:
            if eng != "pe":
                self._resolve(("e", eng, idx))
            else:
                wk = tuple(self._key(t) for t in w)
                if self.pe_prev is not None and self.pe_prev[1] != wk:
                    self._resolve(("e", "pe", self.pe_prev[0]))
                self.pe_prev = (idx, wk)
        if self.log is not None:
            self.log.append("%s[%d] r=%s w=%s" % (eng, idx, [self._key(t) for t in r], [self._key(t) for t in w]))
        ref = ("e", eng, idx)
        self._record(ref, r, w)
        return ref

    def _emit_dma(self, q, key, fn, r=(), w=()):
        self._deps(q, r, w)
        if key not in self.dsem:
            self.dsem[key] = [self.es.enter_context(self.nc.semaphore("dq%d" % len(self.dsem))), 0]
        ent = self.dsem[key]
        inst = fn(self.eng[q])
        inst.then_inc(ent[0], 16)
        ent[1] += 16
        if self.log is not None:
            self.log.append("dma on %s key=%s -> %d r=%s w=%s" % (q, key, ent[1], [self._key(t) for t in r], [self._key(t) for t in w]))
        ref = ("d", key, ent[1])
        self._record(ref, r, w)
        return ref

    def barrier(self, engines=("pe", "dve", "act", "pool", "sp")):
        self.flush()
        for c in engines:
            for e in self.sem:
                if e != c and self.insts[e]:
                    self._wait(c, ("e", e, len(self.insts[e]) - 1))
            for key, ent in self.dsem.items():
                if ent[1]:
                    self._wait(c, ("d", key, ent[1]))
        self.last_w.clear()
        self.readers.clear()

    def final_wait(self, q="sp"):
        self.flush()
        for key, ent in self.dsem.items():
            if ent[1]:
                self._wait(q, ("d", key, ent[1]))


class _Cut(Exception):
    pass


def build_program(dbg=None, stop_after=None, n_blocks=NBLK, n_experts=NE, cut=None):
    nc = bass.Bass("TRN2", target_bir_lowering=False)
    dt_in = lambda n, s: nc.dram_tensor(n, s, F32, kind="ExternalInput").ap()
    x_d = dt_in("x", [SEQ, D])
    w_in_d = dt_in("w_in", [D, IN_COLS])
    w_out_d = dt_in("w_out", [D, D])
    convw_d = dt_in("convw_t", [128, 12, 4])
    rowp_d = dt_in("rowp", [128, NRP])
    rowp2_d = dt_in("rowp2", [128, 2 * D])
    colp_d = dt_in("colp", [128, 4])
    wst_d = dt_in("wst", [128, 4, 128])
    wr_d = dt_in("wr", [D, 72])
    wg_d = dt_in("w_gate", [NE, D, 512])
    wu_d = dt_in("w_up", [NE, D, 512])
    wd_d = dt_in("w_down", [NE, 512, D])
    cst_d = dt_in("cst", [128, NCS, 128])
    out_d = nc.dram_tensor("out", [SEQ, D], F32, kind="ExternalOutput").ap()
    h1_d = nc.dram_tensor("h1_scr", [SEQ, D], F32).ap()
    xs_d = nc.dram_tensor("xs_scr", [NROWS, D], BF16).ap()
    ys_d = nc.dram_tensor("ys_scr", [NROWS, D], BF16).ap()

    dbg_outs = {}

    def stage(n):
        if cut is not None and n == cut:
            raise _Cut()
        if n != 3 and not (20 <= n < 40):
            SCHED[0].label = "s%d" % n
        else:
            SCHED[0].label = "blk"

    SCHED = [None]

    with ExitStack() as es:
        S = Sched(nc, es)
        SCHED[0] = S
        if dbg is not None and "LABELS" in dbg:
            dbg["LABELS"] = S.labels
        if dbg is not None and "LOG" in dbg:
            S.log = dbg["LOG"]
        try:
            _emit(nc, es, S, stage, dbg, dbg_outs, stop_after, n_blocks, n_experts, locals())
        except _Cut:
            S.final_wait()
    return nc, dbg_outs


def _emit(nc, es, S, stage, dbg, dbg_outs, stop_after, n_blocks, n_experts, env):
    x_d, w_in_d, w_out_d, convw_d, rowp_d, rowp2_d, colp_d, wst_d, wr_d, wg_d, wu_d, wd_d, cst_d, out_d, h1_d, xs_d, ys_d = (
        env[k] for k in "x_d w_in_d w_out_d convw_d rowp_d rowp2_d colp_d wst_d wr_d wg_d wu_d wd_d cst_d out_d h1_d xs_d ys_d".split())
    if True:
        V, A, P, PE = EngProxy(nc.vector, "dve"), EngProxy(nc.scalar, "act"), EngProxy(nc.gpsimd, "pool"), EngProxy(nc.tensor, "pe")

        def sb(es_, name, shape, dt=F32):
            return Buf(es_.enter_context(nc.sbuf_tensor("sb_" + name, shape, dt)), name)

        banks = [Buf(es.enter_context(nc.psum_tensor("psb%d" % i, [128, 512], F32)), ("ps", i))
                 for i in range(8)]
        bank_i = [0]

        def ps_next():
            b = banks[bank_i[0] % 8]
            bank_i[0] += 1
            return b

        bankb_i = [0]
        bankt_i = [0]

        bankq_i = [0]
        NB_B, NB_T, NB_Q = BANK_SPLIT

        def ps_blk():
            b = banks[bankb_i[0] % NB_B]
            bankb_i[0] += 1
            return b

        def ps_tile():
            b = banks[NB_B + bankt_i[0] % (NB_T - 1)]
            bankt_i[0] += 1
            return b

        def ps_acc():
            return banks[NB_B + NB_T - 1]

        def ps_q():
            b = banks[NB_B + NB_T + bankq_i[0] % NB_Q]
            bankq_i[0] += 1
            return b

        def dump(name, buf, ap, shape, dt=F32):
            if dbg is None or name not in dbg:
                return
            o = nc.dram_tensor("dbg_" + name, shape, dt, kind="ExternalOutput").ap()
            dbg_outs[name] = o
            S.dma("sp", ("dbg", name), lambda q: q.dma_start(out=o, in_=ap), r=[buf], w=[("dbgd", name)])

        cst = sb(es, "cst", [128, NCS, 128])
        rowp = sb(es, "rowp", [128, NRP])
        colp = sb(es, "colp", [128, 4])
        S.dma("sp", "ld_cst", lambda q: q.dma_start(out=cst[:], in_=cst_d), w=[cst])
        S.dma("sp", "ld_rowp", lambda q: q.dma_start(out=rowp[:], in_=rowp_d), w=[rowp])
        S.dma("sp", "ld_colp", lambda q: q.dma_start(out=colp[:], in_=colp_d), w=[colp])

        def C(n):
            return cst[:, CS[n], :]

        def R(n, lo=0, hi=None):
            o, wd = RP[n]
            hi = wd if hi is None else hi
            return rowp[:, o + lo:o + hi]

        cb = sb(es, "cstb", [128, 3, 128], BF16)
        S.op("dve", lambda: V.tensor_copy(cb[:, 0, :], C("ident")), r=[cst], w=[cb])
        S.op("dve", lambda: V.tensor_copy(cb[:, 1, :], C("ones")), r=[cst], w=[cb])
        S.op("dve", lambda: V.tensor_copy(cb[:, 2, :], C("sltri")), r=[cst], w=[cb])
        identb, onesb, sltrib = cb[:, 0, :], cb[:, 1, :], cb[:, 2, :]
        identf, onesf = C("ident"), C("ones")

        small = sb(es, "small", [128, 16])
        S.op("dve", lambda: V.memset(small[:, 4:5], RMS_EPS), w=[small])
        S.op("dve", lambda: V.memset(small[:, 5:6], LN_EPS), w=[small])
        S.op("dve", lambda: V.memset(small[:, 6:7], float(np.log(128.0 ** -0.5))), w=[small])
        S.op("dve", lambda: V.memset(small[:, 7:8], 1.0), w=[small])
        S.op("dve", lambda: V.memset(small[:, 8:10], 0.0), w=[small])
        S.op("dve", lambda: V.memset(small[0:64, 8:9], 1.0), w=[small])
        S.op("dve", lambda: V.memset(small[64:128, 9:10], 1.0), w=[small])
        S.op("act", lambda: A.activation(out=small[:, 0:4], in_=R("a_log"), func=AF.Exp), r=[rowp, small], w=[small])
        S.op("dve", lambda: V.tensor_scalar(small[:, 0:4], small[:, 0:4], -1.0, None, ALU.mult), r=[small], w=[small])
        EPS_RMS, EPS_LN, LNQS, ONE = small[:, 4:5], small[:, 5:6], small[:, 6:7], small[:, 7:8]
        CM = [small[:, 8:9], small[:, 9:10]]

        RI = sb(es, "RI", [128, NT, 4])
        RIi = sb(es, "RIi", [128, NT, 2], I32)

        es1 = es.enter_context(ExitStack())
        w_in_b = sb(es1, "w_in_b", [128, 8, IN_COLS], BF16)
        w_out_b = sb(es1, "w_out_b", [128, 8, D], BF16)
        wrt = sb(es1, "wrt", [128, 8, 72])
        convw = sb(es1, "convw", [128, 12, 4])
        cdiag = sb(es1, "cdiag", [128, 48, 128], BF16)
        wstb = sb(es1, "wstb", [128, 4, 128], BF16)
        w_in_v = w_in_d.rearrange("(kc p) c -> p kc c", p=128)
        for kc in range(8):
            S.dma("pool", ("ld_win", kc), lambda q, kc=kc: q.dma_start(out=w_in_b[:, kc, :], in_=w_in_v[:, kc, :]), w=[w_in_b])
        S.dma("sp", "ld_convw", lambda q: q.dma_start(out=convw[:], in_=convw_d), w=[convw])
        S.dma("pool", "ld_wst", lambda q: q.dma_start(out=wstb[:], in_=wst_d), w=[wstb])
        S.dma("sp", "ld_wr", lambda q: q.dma_start(out=wrt[:], in_=wr_d.rearrange("(kc p) c -> p kc c", p=128)), w=[wrt])
        w_out_v = w_out_d.rearrange("(kc p) c -> p kc c", p=128)
        for kc in range(0, 8, 4):
            S.dma("pool", ("ld_wout", kc), lambda q, kc=kc: q.dma_start(out=w_out_b[:, kc:kc + 4, :], in_=w_out_v[:, kc:kc + 4, :]), w=[w_out_b])
        for cc in range(12):
            for j in range(4):
                S.op("dve", lambda cc=cc, j=j: V.tensor_scalar(cdiag[:, cc * 4 + j, :], identf, convw[:, cc, j:j + 1], None, ALU.mult),
                     r=[cst, convw], w=[cdiag])
        S.op("dve", lambda: V.tensor_tensor(wstb[:], wstb[:], C("maskWS").unsqueeze(1).broadcast_to([128, 4, 128]), ALU.mult),
             r=[wstb, cst], w=[wstb])

        stage(1)
        xsl = [sb(es1, "xsl%d" % i, [128, D], BF16) for i in range(2)]
        xr = [sb(es1, "xr%d" % i, [128, D]) for i in range(1)]
        xTb = sb(es1, "xTb", [128, 8, BW], BF16)
        eB = [sb(es1, "eB%d" % i, [128, 512]) for i in range(2)]
        sp8 = [sb(es1, "sp8_%d" % i, [128, 8]) for i in range(2 * TPB)]
        raw = [sb(es1, "raw%d" % i, [128, BW + 3], BF16) for i in range(2)]
        halo = sb(es1, "halo", [128, 12, 3], BF16)
        qs = [sb(es1, "qs%d" % i, [128, BW]) for i in range(1)]
        sqb = [sb(es1, "sqb%d" % i, [128, 512], BF16) for i in range(1)]
        sqbB = [sb(es1, "sqbB%d" % i, [128, BW], BF16) for i in range(2)]
        tmpB = [sb(es1, "tmpB%d" % i, [128, BW]) for i in range(2)]
        qT2 = [sb(es1, "qT%d" % i, [128, 4, BW], BF16) for i in range(2)]
        kT2 = [sb(es1, "kT%d" % i, [128, 4, BW], BF16) for i in range(2)]
        vT2 = [sb(es1, "vT%d" % i, [128, 4, BW], BF16) for i in range(2)]
        ZG2 = [sb(es1, "ZG%d" % i, [128, 4, BW], BF16) for i in range(2)]
        UG2 = [sb(es1, "UG%d" % i, [128, 4, BW], BF16) for i in range(2)]
        YT = sb(es1, "YT", [128, 8, BW], BF16)
        XC = sb(es1, "XC", [128, 4, 128])
        VLN = [sb(es1, "VLN%d" % i, [128, 512], BF16) for i in range(2 * TPB)]
        st4 = sb(es1, "st4", [128, 16])
        st4B = sb(es1, "st4B", [128, 16])
        cf2 = [sb(es1, "cf%d" % i, [128, 64]) for i in range(2)]
        Dg = sb(es1, "Dg", [128, 4, 128])
        X1 = Dg
        X3 = sb(es1, "X3", [128, 4, 128])
        E1, E3 = X1, X3
        tQ1 = sb(es1, "tQ1", [128, 512])
        tQ2 = tQ1
        EG2 = [sb(es1, "EG%d" % i, [128, 4, 128]) for i in range(2)]
        Mb = [sb(es1, "Mb%d" % i, [128, 4, 128], BF16) for i in range(2)]
        MTb = [sb(es1, "MTb%d" % i, [128, 4, 128], BF16) for i in range(2)]
        Pb = [sb(es1, "Pb%d" % i, [128, 4, 128], BF16) for i in range(2)]
        AinT2 = [sb(es1, "AinT%d" % i, [128, 4, 128], BF16) for i in range(2)]
        QDT2 = [sb(es1, "QDT%d" % i, [128, 4, 128], BF16) for i in range(2)]
        RHSw = sb(es1, "RHSw", [128, 4, 128], BF16)
        RHSu = sb(es1, "RHSu", [128, 4, 128], BF16)
        KD2 = [[sb(es1, "KD%d_%d" % (j, i), [128, 4, 128], BF16) for i in range(2)] for j in range(2)]
        Uf2 = [sb(es1, "Uf%d" % i, [128, 4, 128]) for i in range(2)]
        WTb2 = [sb(es1, "WTb%d" % i, [128, 4, 128], BF16) for i in range(2)]
        VN = sb(es1, "VN", [128, 4, 128], BF16)
        Sf = sb(es1, "Sf", [128, 4, 128])
        Sb = sb(es1, "Sb", [128, 4, 128], BF16)
        OT = sb(es1, "OT", [128, 4, 128])
        hp = xr[0]
        h1 = [sb(es1, "h1_%d" % i, [128, D]) for i in range(1)]
        h1b = [sb(es1, "h1b_%d" % i, [128, D], BF16) for i in range(1)]
        h1T = sb(es1, "h1T", [128, 8, 128])
        junk = Buf(h1T.t[:].rearrange("p k j -> p (k j)"), h1T.k)
        rt = sb(es1, "rt", [128, 512])
        OHb = sb(es1, "OHb", [128, 64], BF16)
        carry = sb(es1, "carry", [128, 64])
        zero_b = Buf(rt.t[:].bitcast(BF16).rearrange("p (o d) -> p o d", o=1), rt.k)

        S.op("dve", lambda: V.memset(Sf[:], 0.0), w=[Sf])
        S.op("dve", lambda: V.memset(Sb[:], 0.0), w=[Sb])
        S.op("dve", lambda: V.memset(VN[:], 0.0), w=[VN])
        S.op("dve", lambda: V.memset(halo[:], 0.0), w=[halo])
        S.op("dve", lambda: V.memset(carry[:], 0.0), w=[carry])
        S.op("pool", lambda: P.memset(rt[:], 0.0), w=[rt])

        def ln_rows(src, dst, g_ap, b_ap, scr, gbuf=None):
            gbuf = rowp if gbuf is None else gbuf
            S.op("dve", lambda: V.tensor_reduce(scr[:, 0:1], src[:], AX.X, ALU.add), r=[src], w=[scr])
            yield
            S.op("dve", lambda: V.tensor_scalar(scr[:, 1:2], scr[:, 0:1], -1.0 / D, None, ALU.mult), r=[scr], w=[scr])
            yield
            S.op("act", lambda: A.activation(out=junk[:], in_=src[:], func=AF.Square, bias=scr[:, 1:2], scale=1.0,
                                             accum_out=scr[:, 2:3]), r=[src, scr], w=[junk, scr])
            yield
            S.op("act", lambda: A.activation(out=scr[:, 3:4], in_=scr[:, 2:3], func=AF.Ln, bias=EPS_LN, scale=1.0 / D),
                 r=[scr, small], w=[scr])
            yield
            S.op("act", lambda: A.activation(out=scr[:, 4:5], in_=scr[:, 3:4], func=AF.Exp, scale=-0.5), r=[scr], w=[scr])
            yield
            S.op("dve", lambda: V.tensor_tensor(scr[:, 5:6], scr[:, 1:2], scr[:, 4:5], ALU.mult), r=[scr], w=[scr])
            yield
            S.op("act", lambda: A.activation(out=dst[:], in_=src[:], func=AF.Identity, bias=scr[:, 5:6], scale=scr[:, 4:5]), r=[src, scr], w=[dst])
            yield
            S.op("pool", lambda: P.tensor_tensor(dst[:], dst[:], g_ap, ALU.mult), r=[dst, gbuf], w=[dst])
            yield
            S.op("dve", lambda: V.tensor_tensor(dst[:], dst[:], b_ap, ALU.add), r=[dst, gbuf], w=[dst])
            yield

        x_t = x_d.rearrange("(n p) d -> n p d", p=128)
        h1_t = h1_d.rearrange("(n p) d -> n p d", p=128)
        out_t = out_d.rearrange("(n p) d -> n p d", p=128)

        def bc_h(ap2):
            return ap2.unsqueeze(1).broadcast_to([128, 4, 128])

        def bc_j(ap2):
            return ap2.unsqueeze(2).broadcast_to([128, 4, 128])

        def v3(b):
            return b[:].rearrange("p (h j) -> p h j", h=4)

        def load_x(ti):
            S.dma("pool", ("x", ti % 2), lambda q: q.dma_start(out=xsl[ti % 2][:], in_=x_t[ti]), w=[xsl[ti % 2]])

        zero_done = [0]

        def zero_fill(n):
            zr = xs_d.rearrange("(n r p) d -> n p r d", p=128, r=1)
            tot = NROWS // 128
            for _ in range(n):
                i = zero_done[0]
                if i < tot:
                    wt = [("xs_zero", i)] + ([("xs_zero_last", i % 4)] if i >= tot - 4 + 1 else [])
                    S.dma("sp", ("zf", i % 4), lambda q, i=i: q.dma_start(out=zr[i], in_=zero_b[:]), r=[zero_b], w=wt)
                elif i == tot:
                    S.dma("sp", ("zf", i % 4), lambda q: q.dma_start(out=ys_d[TRASH:TRASH + 128, :], in_=zero_b[:, 0, :]), r=[zero_b], w=["ys_trash", ("xs_zero_last", i % 4)])
                zero_done[0] += 1
        assert NROWS % 128 == 0

        stage(2)
        for ti in range(2):
            load_x(ti)

        def act_sigmoid(dst_ap, dst_buf, src_ap, src_buf, scale=1.0):
            S.op("act", lambda: A.activation(out=dst_ap, in_=src_ap, func=AF.Exp, scale=-scale), r=[src_buf], w=[dst_buf])
            S.op("act", lambda: A.activation(out=dst_ap, in_=dst_ap, func=AF.Ln, bias=ONE, scale=1.0), r=[dst_buf, small], w=[dst_buf])
            S.op("act", lambda: A.activation(out=dst_ap, in_=dst_ap, func=AF.Exp, scale=-1.0), r=[dst_buf], w=[dst_buf])

        GC_A = float(np.sqrt(0.044715))
        GC_B = float(2.0 * np.sqrt(2.0 / np.pi))

        def gelu_tanh(dst_ap, dst_buf, src_ap, src_buf, e_ap, e_buf):
            S.op("act", lambda: A.activation(out=e_ap, in_=src_ap, func=AF.Square, scale=GC_A), r=[src_buf], w=[e_buf])
            S.op("dve", lambda: V.scalar_tensor_tensor(e_ap, e_ap, 1.0, src_ap, ALU.add, ALU.mult), r=[e_buf, src_buf], w=[e_buf])
            act_sigmoid(e_ap, e_buf, e_ap, e_buf, scale=GC_B)
            S.op("dve", lambda: V.tensor_tensor(dst_ap, src_ap, e_ap, ALU.mult), r=[src_buf, e_buf], w=[dst_buf])

        def block_gen(blk):
            bs = blk % 2
            qT, kT, vT, ZG, UG = qT2[bs], kT2[bs], vT2[bs], ZG2[bs], UG2[bs]
            for tl in range(TPB):
                ti = blk * TPB + tl
                xs_ = xsl[ti % 2]
                pb = ps_blk()
                pb_b = pb[:].bitcast(BF16)
                for kc in range(8):
                    S.op("pe", lambda: PE.transpose(pb_b[:, kc * 128:(kc + 1) * 128], xs_[:, kc * 128:(kc + 1) * 128], identb),
                         r=[xs_, cb], w=[pb])
                yield
                S.op("act", lambda: A.activation(out=xTb[:, :, tl * 128:(tl + 1) * 128], in_=pb_b.rearrange("p (k j) -> p k j", k=8), func=AF.Copy),
                     r=[pb], w=[xTb])
                if ti + 2 < n_blocks * TPB:
                    load_x(ti + 2)
                yield
                pb = ps_blk()
                for kc in range(8):
                    S.op("pe", lambda: PE.matmul(pb[:, 0:8], xTb[:, kc, tl * 128:(tl + 1) * 128], w_in_b[:, kc, C_SP:C_SP + 8], start=(kc == 0), stop=(kc == 7)),
                         r=[xTb, w_in_b], w=[pb])
                yield
                S.op("dve", lambda: V.tensor_copy(sp8[bs * TPB + tl][:], pb[:, 0:8]), r=[pb], w=[sp8[bs * TPB + tl]])
                yield
            if blk == 0 and ZF:
                zero_fill(NROWS // 128 + 1)

            stage(3)
            yield
            def proj_chunk(col0):
                pb_ = ps_blk()
                for kc in range(8):
                    S.op("pe", lambda: PE.matmul(pb_[:, 0:BW], w_in_b[:, kc, col0:col0 + 128], xTb[:, kc, :], start=(kc == 0), stop=(kc == 7)),
                         r=[w_in_b, xTb], w=[pb_])
                return pb_

            for cc in range(12):
                pb = proj_chunk(cc * 128)
                yield
                rw = raw[cc % 2]
                S.op("pool", lambda: P.tensor_copy(rw[:, 0:3], halo[:, cc, :]), r=[halo], w=[rw])
                S.op("act", lambda: A.activation(out=rw[:, 3:BW + 3], in_=pb[:, 0:BW], func=AF.Copy), r=[pb], w=[rw])
                S.op("pool", lambda: P.tensor_copy(halo[:, cc, :], rw[:, BW:BW + 3]), r=[rw], w=[halo])
                pc = ps_blk()
                for j in range(4):
                    S.op("pe", lambda: PE.matmul(pc[:, 0:BW], cdiag[:, cc * 4 + j, :], rw[:, j:j + BW], start=(j == 0), stop=(j == 3)),
                         r=[cdiag, rw], w=[pc])
                yield
                hh = cc % 4
                if cc < 8:
                    q_ = qs[0]
                    sq_ = sqbB[cc % 2]
                    t_ = tmpB[cc % 2]
                    dstT = qT if cc < 4 else kT
                    e_ = eB[cc % 2]
                    act_sigmoid(e_[:, 0:BW], e_, pc[:, 0:BW], pc)
                    S.op("dve", lambda: V.tensor_tensor(q_[:, 0:BW], pc[:, 0:BW], e_[:, 0:BW], ALU.mult), r=[pc, e_], w=[q_])
                    S.op("pool", lambda: P.tensor_tensor(sq_[:, 0:BW], q_[:, 0:BW], q_[:, 0:BW], ALU.mult), r=[q_], w=[sq_])
                    pn = ps_blk()
                    S.op("pe", lambda: PE.matmul(pn[:, 0:BW], onesb, sq_[:, 0:BW], start=True, stop=True), r=[cb, sq_], w=[pn])
                    yield
                    S.op("act", lambda: A.activation(out=t_[:, 0:BW], in_=pn[:, 0:BW], func=AF.Ln, bias=EPS_RMS, scale=1.0), r=[pn, small], w=[t_])
                    if cc < 4:
                        S.op("act", lambda: A.activation(out=t_[:, 0:BW], in_=t_[:, 0:BW], func=AF.Exp, bias=LNQS, scale=-0.5), r=[t_, small], w=[t_])
                    else:
                        S.op("act", lambda: A.activation(out=t_[:, 0:BW], in_=t_[:, 0:BW], func=AF.Exp, scale=-0.5), r=[t_], w=[t_])
                    yield
                    S.op("dve", lambda: V.tensor_tensor(dstT[:, hh, :], q_[:, 0:BW], t_[:, 0:BW], ALU.mult), r=[q_, t_], w=[dstT])
                else:
                    e_ = eB[cc % 2]
                    act_sigmoid(e_[:, 0:BW], e_, pc[:, 0:BW], pc)
                    S.op("dve", lambda: V.tensor_tensor(vT[:, hh, :], pc[:, 0:BW], e_[:, 0:BW], ALU.mult), r=[pc, e_], w=[vT])
                yield
            for zc in range(4):
                pb = proj_chunk(C_Z + zc * 128)
                yield
                t_ = tmpB[zc % 2]
                act_sigmoid(t_[:, 0:BW], t_, pb[:, 0:BW], pb)
                S.op("dve", lambda: V.scalar_tensor_tensor(ZG[:, zc, :], t_[:, 0:BW], colp[:, 0:1], pb[:, 0:BW], ALU.mult, ALU.mult), r=[t_, colp, pb], w=[ZG])
                yield
            for uc in range(4):
                pb = proj_chunk(C_U + uc * 128)
                yield
                e_ = eB[uc % 2]
                gelu_tanh(UG[:, uc, :], UG, pb[:, 0:BW], pb, e_[:, 0:BW], e_)
                yield
            if blk == 0:
                dump("qT", qT, qT[:], [128, 4, BW], BF16)
                dump("kT", kT, kT[:], [128, 4, BW], BF16)
                dump("vT", vT, vT[:], [128, 4, BW], BF16)

            stage(4)
            yield
            for tl in range(TPB):
                pb = ps_blk()
                for kc in range(8):
                    S.op("pe", lambda: PE.matmul(pb[:], xTb[:, kc, tl * 128:(tl + 1) * 128], w_in_b[:, kc, C_VS:C_VS + 512],
                                                 start=(kc == 0), stop=(kc == 7)), r=[xTb, w_in_b], w=[pb])
                yield
                e_ = eB[tl % 2]
                VG = Buf(e_.t[:].rearrange("p (h j) -> p h j", h=4), e_.k)
                gelu_tanh(VG[:], VG, v3(pb), pb, VG[:], VG)
                S.op("dve", lambda: V.tensor_reduce(st4B[:, 0:4], VG[:], AX.X, ALU.add), r=[VG], w=[st4B])
                S.op("dve", lambda: V.tensor_scalar(st4B[:, 4:8], st4B[:, 0:4], -1.0 / 128, None, ALU.mult), r=[st4B], w=[st4B])
                S.op("dve", lambda: V.tensor_tensor(XC[:], VG[:], bc_j(st4B[:, 4:8]), ALU.add), r=[VG, st4B], w=[XC])
                yield
                S.op("pool", lambda: P.tensor_tensor(VG[:], XC[:], XC[:], ALU.mult), r=[XC], w=[VG])
                S.op("dve", lambda: V.tensor_reduce(st4B[:, 8:12], VG[:], AX.X, ALU.add), r=[VG], w=[st4B])
                yield
                S.op("act", lambda: A.activation(out=st4B[:, 8:12], in_=st4B[:, 8:12], func=AF.Ln, bias=EPS_LN, scale=1.0 / 128), r=[st4B, small], w=[st4B])
                S.op("act", lambda: A.activation(out=st4B[:, 12:16], in_=st4B[:, 8:12], func=AF.Exp, scale=-0.5), r=[st4B], w=[st4B])
                S.op("dve", lambda: V.tensor_tensor(XC[:], XC[:], bc_j(st4B[:, 12:16]), ALU.mult), r=[XC, st4B], w=[XC])
                yield
                S.op("pool", lambda: P.tensor_tensor(XC[:], XC[:], R("sgu_g").rearrange("p (g d) -> p g d", g=4), ALU.mult), r=[XC, rowp], w=[XC])
                S.op("dve", lambda: V.tensor_tensor(VLN[bs * TPB + tl][:].rearrange("p (g d) -> p g d", g=4), XC[:], R("sgu_b").rearrange("p (g d) -> p g d", g=4), ALU.add),
                     r=[XC, rowp], w=[VLN[bs * TPB + tl]])
                yield

            if blk == 0:
                dump("VLN0", VLN[bs * TPB], VLN[bs * TPB][:], [128, 512], BF16)
        def tile_gen(blk, tl):
            if True:
                stage(5)
                bs = blk % 2
                qT, kT, vT, ZG, UG = qT2[bs], kT2[bs], vT2[bs], ZG2[bs], UG2[bs]
                ti = blk * TPB + tl
                par = ti % 2
                cf, EG, AinT, QDT, KD, Uf, WTb = cf2[par], EG2[par], AinT2[par], QDT2[par], KD2[par], Uf2[par], WTb2[par]
                tc0 = tl * 128
                tcs = slice(tc0, tc0 + 128)
                s8 = sp8[bs * TPB + tl]
                cfk = [cf]
                S.op("act", lambda: A.activation(out=cf[:, 0:4], in_=s8[:, 0:4], func=AF.Exp, scale=-1.0), r=[s8], w=cfk)
                S.op("act", lambda: A.activation(out=cf[:, 0:4], in_=cf[:, 0:4], func=AF.Ln, bias=ONE, scale=1.0), r=[cf, small], w=cfk)
                S.op("dve", lambda: V.tensor_scalar(cf[:, 0:4], cf[:, 0:4], -1.0, None, ALU.mult), r=cfk, w=cfk)
                yield
                S.op("act", lambda: A.activation(out=cf[:, 4:8], in_=cf[:, 0:4], func=AF.Exp), r=cfk, w=cfk)
                S.op("dve", lambda: V.tensor_tensor(cf[:, 8:12], s8[:, 4:8], R("dt_bias"), ALU.add), r=[s8, rowp], w=cfk)
                S.op("act", lambda: A.activation(out=cf[:, 8:12], in_=cf[:, 8:12], func=AF.Exp), r=cfk, w=cfk)
                yield
                S.op("act", lambda: A.activation(out=cf[:, 8:12], in_=cf[:, 8:12], func=AF.Ln, bias=ONE, scale=1.0), r=[cf, small], w=cfk)
                S.op("dve", lambda: V.tensor_tensor(cf[:, 8:12], cf[:, 8:12], small[:, 0:4], ALU.mult), r=[cf, small], w=cfk)
                pb = ps_tile()
                S.op("pe", lambda: PE.matmul(pb[:, 0:4], C("tri"), cf[:, 8:12], start=True, stop=True), r=[cst, cf], w=[pb])
                S.op("pe", lambda: PE.matmul(pb[:, 4:8], C("chs"), cf[:, 8:12], start=True, stop=True), r=[cst, cf], w=[pb])
                yield
                S.op("dve", lambda: V.tensor_copy(cf[:, 12:20], pb[:, 0:8]), r=[pb], w=cfk)
                S.op("dve", lambda: V.tensor_tensor(cf[:, 20:24], cf[:, 12:16], cf[:, 0:4], ALU.add), r=cfk, w=cfk)
                S.op("dve", lambda: V.tensor_scalar(cf[:, 24:28], cf[:, 12:16], -1.0, None, ALU.mult), r=cfk, w=cfk)
                yield
                S.op("act", lambda: A.activation(out=cf[:, 28:32], in_=cf[:, 12:16], func=AF.Exp), r=cfk, w=cfk)
                S.op("dve", lambda: V.tensor_tensor(cf[:, 32:36], cf[:, 4:8], cf[:, 28:32], ALU.mult), r=cfk, w=cfk)
                S.op("dve", lambda: V.tensor_tensor(cf[:, 36:40], cf[:, 16:20], cf[:, 12:16], ALU.subtract), r=cfk, w=cfk)
                yield
                S.op("act", lambda: A.activation(out=cf[:, 36:40], in_=cf[:, 36:40], func=AF.Exp), r=cfk, w=cfk)
                S.op("dve", lambda: V.tensor_scalar(cf[:, 40:44], cf[:, 36:40], CM[0], None, ALU.mult), r=[cf, small], w=cfk)
                S.op("dve", lambda: V.tensor_scalar(cf[:, 44:48], cf[:, 36:40], CM[1], None, ALU.mult), r=[cf, small], w=cfk)
                GC, A_, NGC, BETA, CW = cf[:, 12:16], cf[:, 20:24], cf[:, 24:28], cf[:, 4:8], cf[:, 32:36]
                if ti == 0:
                    dump("cf", cf, cf[:], [128, 64])
                stage(6)
                yield
                S.op("dve", lambda: V.tensor_tensor(Dg[:], bc_h(identf), bc_j(GC), ALU.mult), r=[cst, cf], w=[Dg])
                pgc = ps_tile()
                S.op("pe", lambda: PE.matmul(pgc[:], onesf, Dg[:].rearrange("p h j -> p (h j)"), start=True, stop=True), r=[cst, Dg], w=[pgc])
                yield
                S.op("dve", lambda: V.scalar_tensor_tensor(X1[:], v3(pgc), -1.0, bc_h(C("maskS")), ALU.mult, ALU.add), r=[pgc, cst], w=[X1])
                S.op("dve", lambda: V.tensor_tensor(X3[:], v3(pgc), bc_h(C("maskIT")), ALU.add), r=[pgc, cst], w=[X3])
                S.op("act", lambda: A.activation(out=EG[:], in_=v3(pgc), func=AF.Exp), r=[pgc], w=[EG])
                for h in range(4):
                    yield
                    S.op("act", lambda: A.activation(out=E1[:, h, :], in_=X1[:, h, :], func=AF.Exp, bias=cf[:, 20 + h:21 + h], scale=1.0), r=[X1, cf], w=[E1])
                    S.op("act", lambda: A.activation(out=E3[:, h, :], in_=X3[:, h, :], func=AF.Exp, bias=cf[:, 24 + h:25 + h], scale=1.0), r=[X3, cf], w=[E3])
                if ti == 0:
                    dump("E1", E1, E1[:], [128, 4, 128])
                    dump("E3", E3, E3[:], [128, 4, 128])
                    dump("EG", EG, EG[:], [128, 4, 128])
                stage(7)
                yield
                pk = ps_tile()
                for h in range(4):
                    S.op("pe", lambda: PE.matmul(pk[:, h * 128:(h + 1) * 128], kT[:, h, tcs], kT[:, h, tcs], start=True, stop=True), r=[kT], w=[pk])
                yield
                S.op("dve", lambda: V.scalar_tensor_tensor(Mb[0][:], v3(pk), -1.0, E1[:], ALU.mult, ALU.mult), r=[pk, E1], w=[Mb[0]])
                ptm = ps_tile()
                ptm_b = ptm[:].bitcast(BF16)
                for h in range(4):
                    S.op("pe", lambda: PE.transpose(ptm_b[:, h * 128:(h + 1) * 128], Mb[0][:, h, :], identb), r=[Mb[0], cb], w=[ptm])
                yield
                S.op("act", lambda: A.activation(out=MTb[0][:], in_=ptm_b[:, 0:512].rearrange("p (h j) -> p h j", h=4), func=AF.Copy), r=[ptm], w=[MTb[0]])
                S.op("pool", lambda: P.tensor_tensor(Pb[0][:], MTb[0][:], bc_h(identb), ALU.add), r=[MTb[0], cb], w=[Pb[0]])
                pq = ps_tile()
                for h in range(4):
                    S.op("pe", lambda: PE.matmul(pq[:, h * 128:(h + 1) * 128], kT[:, h, tcs], qT[:, h, tcs], start=True, stop=True), r=[kT, qT], w=[pq])
                yield
                S.op("dve", lambda: V.tensor_tensor(AinT[:], v3(pq), E3[:], ALU.mult), r=[pq, E3], w=[AinT])
                S.op("pool", lambda: P.tensor_tensor(QDT[:], qT[:, :, tcs], EG[:], ALU.mult), r=[qT, EG], w=[QDT])
                ptk = ps_tile()
                ptv = ps_tile()
                ptk_b = ptk[:].bitcast(BF16)
                ptv_b = ptv[:].bitcast(BF16)
                for h in range(4):
                    S.op("pe", lambda: PE.transpose(ptk_b[:, h * 128:(h + 1) * 128], kT[:, h, tcs], identb), r=[kT, cb], w=[ptk])
                    S.op("pe", lambda: PE.transpose(ptv_b[:, h * 128:(h + 1) * 128], vT[:, h, tcs], identb), r=[vT, cb], w=[ptv])
                k3 = ptk_b[:, 0:512].rearrange("p (h j) -> p h j", h=4)
                vv3 = ptv_b[:, 0:512].rearrange("p (h j) -> p h j", h=4)
                yield
                S.op("dve", lambda: V.tensor_tensor(RHSw[:], k3, bc_j(CW), ALU.mult), r=[ptk, cf], w=[RHSw])
                S.op("dve", lambda: V.tensor_tensor(KD[0][:], k3, bc_j(cf[:, 40:44]), ALU.mult), r=[ptk, cf], w=[KD[0]])
                S.op("dve", lambda: V.tensor_tensor(KD[1][:], k3, bc_j(cf[:, 44:48]), ALU.mult), r=[ptk, cf], w=[KD[1]])
                yield
                S.op("dve", lambda: V.tensor_tensor(RHSu[:], vv3, bc_j(BETA), ALU.mult), r=[ptv, cf], w=[RHSu])
                if ti == 0:
                    dump("M0", Mb[0], Mb[0][:], [128, 4, 128], BF16)
                    dump("MT0", MTb[0], MTb[0][:], [128, 4, 128], BF16)
                    dump("AinT", AinT, AinT[:], [128, 4, 128], BF16)
                    dump("RHSu", RHSu, RHSu[:], [128, 4, 128], BF16)
                    dump("RHSw", RHSw, RHSw[:], [128, 4, 128], BF16)
                stage(8)
                yield
                for s_ in range(6):
                    cur, nxt = s_ % 2, (s_ + 1) % 2
                    if s_ >= 1:
                        pP = ps_tile()
                        for h in range(4):
                            S.op("pe", lambda: PE.matmul(pP[:, h * 128:(h + 1) * 128], Mb[cur][:, h, :], Pb[nxt][:, h, :], start=True, stop=True),
                                 r=[Mb[cur], Pb[nxt]], w=[pP])
                    if s_ < 5:
                        pM = ps_tile()
                        for h in range(4):
                            S.op("pe", lambda: PE.matmul(pM[:, h * 128:(h + 1) * 128], MTb[cur][:, h, :], Mb[cur][:, h, :], start=True, stop=True),
                                 r=[Mb[cur], MTb[cur]], w=[pM])
                    if s_ < 4:
                        pMT = ps_tile()
                        for h in range(4):
                            S.op("pe", lambda: PE.matmul(pMT[:, h * 128:(h + 1) * 128], Mb[cur][:, h, :], MTb[cur][:, h, :], start=True, stop=True),
                                 r=[Mb[cur], MTb[cur]], w=[pMT])
                    if s_ >= 1:
                        yield
                        S.op("dve", lambda: V.tensor_tensor(Pb[cur][:], Pb[nxt][:], v3(pP), ALU.add), r=[Pb[nxt], pP], w=[Pb[cur]])
                    if s_ < 5:
                        S.op("act", lambda: A.activation(out=Mb[nxt][:], in_=v3(pM), func=AF.Copy), r=[pM], w=[Mb[nxt]])
                    if s_ < 4:
                        S.op("dve", lambda: V.tensor_copy(MTb[nxt][:], v3(pMT)), r=[pMT], w=[MTb[nxt]])
                    yield
                stage(9)
                yield
                TT = Pb[1]
                pu = ps_tile()
                pw = ps_tile()
                for h in range(4):
                    S.op("pe", lambda: PE.matmul(pu[:, h * 128:(h + 1) * 128], TT[:, h, :], RHSu[:, h, :], start=True, stop=True), r=[TT, RHSu], w=[pu])
                    S.op("pe", lambda: PE.matmul(pw[:, h * 128:(h + 1) * 128], RHSw[:, h, :], TT[:, h, :], start=True, stop=True), r=[TT, RHSw], w=[pw])
                yield
                S.op("act", lambda: A.activation(out=Uf[:], in_=v3(pu), func=AF.Copy), r=[pu], w=[Uf])
                S.op("act", lambda: A.activation(out=WTb[:], in_=v3(pw), func=AF.Copy), r=[pw], w=[WTb])
                if ti == 0:
                    dump("TT", TT, TT[:], [128, 4, 128], BF16)
                    dump("Uf", Uf, Uf[:], [128, 4, 128])
                    dump("WTb", WTb, WTb[:], [128, 4, 128], BF16)
                po = ps_acc()
                for c in range(2):
                    cs_ = slice(c * 64, c * 64 + 64)
                    pws = ps_tile()
                    for h in range(4):
                        S.op("pe", lambda: PE.matmul(pws[:, h * 128:(h + 1) * 128], WTb[:, h, :], Sb[:, h, :], start=True, stop=True), r=[WTb, Sb], w=[pws])
                    yield
                    S.op("dve", lambda: V.tensor_tensor(VN[:], Uf[:], v3(pws), ALU.subtract), r=[Uf, pws], w=[VN])
                    yield
                    for h in range(4):
                        oc = slice(h * 128 + c * 64, h * 128 + c * 64 + 64)
                        S.op("pe", lambda: PE.matmul(po[:, oc], Sb[:, h, :], QDT[:, h, cs_], start=True, stop=False), r=[Sb, QDT], w=[po])
                        S.op("pe", lambda: PE.matmul(po[:, oc], VN[:, h, :], AinT[:, h, cs_], start=False, stop=True), r=[VN, AinT], w=[po])
                    pS = ps_tile()
                    for h in range(4):
                        S.op("pe", lambda: PE.matmul(pS[:, h * 128:(h + 1) * 128], KD[c][:, h, :], VN[:, h, :], start=True, stop=True), r=[KD[c], VN], w=[pS])
                    for h in range(4):
                        gl = EG[:, h, c * 64 + 63:c * 64 + 64]
                        yield
                        S.op("dve", lambda: V.scalar_tensor_tensor(Sf[:, h, :], Sf[:, h, :], gl, pS[:, h * 128:(h + 1) * 128], ALU.mult, ALU.add),
                             r=[Sf, EG, pS], w=[Sf])
                    S.op("act", lambda: A.activation(out=Sb[:], in_=Sf[:], func=AF.Copy), r=[Sf], w=[Sb])
                    yield
                S.op("act", lambda: A.activation(out=OT[:], in_=v3(po), func=AF.Copy), r=[po], w=[OT])
                if ti == 0:
                    dump("OT0", OT, OT[:], [128, 4, 128])
                if ti == 1:
                    dump("OT1", OT, OT[:], [128, 4, 128])
                stage(10)
                yield "Q"
                sq_ = sqb[0]
                S.op("pool", lambda: P.tensor_tensor(sq_[:], OT[:].rearrange("p h j -> p (h j)"), OT[:].rearrange("p h j -> p (h j)"), ALU.mult), r=[OT], w=[sq_])
                pss = ps_q()
                S.op("pe", lambda: PE.matmul(pss[:], onesb, sq_[:], start=True, stop=True), r=[cb, sq_], w=[pss])
                t_ = tQ1
                yield
                S.op("act", lambda: A.activation(out=t_[:], in_=pss[:], func=AF.Ln, bias=EPS_RMS, scale=1.0 / 128), r=[pss, small], w=[t_])
                S.op("act", lambda: A.activation(out=t_[:], in_=t_[:], func=AF.Exp, scale=-0.5), r=[t_], w=[t_])
                S.op("dve", lambda: V.tensor_tensor(t_[:], t_[:], OT[:].rearrange("p h j -> p (h j)"), ALU.mult), r=[t_, OT], w=[t_])
                yield
                S.op("pool", lambda: P.tensor_tensor(YT[:, 0:4, tcs], t_[:].rearrange("p (h j) -> p h j", h=4), ZG[:, :, tcs], ALU.mult), r=[t_, ZG], w=[YT])
                stage(11)
                yield
                pm = ps_q()
                for g in range(4):
                    S.op("pe", lambda: PE.matmul(pm[:, g * 128:(g + 1) * 128], VLN[bs * TPB + tl][:, g * 128:(g + 1) * 128], wstb[:, g, :], start=True, stop=True),
                         r=[VLN[bs * TPB + tl], wstb], w=[pm])
                t2_ = tQ2
                yield
                S.op("dve", lambda: V.tensor_tensor(t2_[:], pm[:], R("bsp"), ALU.add), r=[pm, rowp], w=[t2_])
                S.op("pool", lambda: P.tensor_tensor(YT[:, 4:8, tcs], t2_[:].rearrange("p (h j) -> p h j", h=4), UG[:, :, tcs], ALU.mult), r=[t2_, UG], w=[YT])
                if ti == 0:
                    dump("YT0", YT, YT[:, :, 0:128], [128, 8, 128], BF16)
                stage(12)
                yield
                xr_ = xr[0]
                S.dma("sp", ("xr", 0), lambda q: q.dma_start(out=xr_[:], in_=x_t[ti]), w=[xr_])
                for half in range(2):
                    px = ps_q()
                    for c8 in range(8):
                        S.op("pe", lambda: PE.matmul(px[:], YT[:, c8, tcs], w_out_b[:, c8, half * 512:(half + 1) * 512], start=(c8 == 0), stop=(c8 == 7)),
                             r=[YT, w_out_b], w=[px])
                    yield
                    S.op("dve", lambda: V.scalar_tensor_tensor(hp[:, half * 512:(half + 1) * 512], xr_[:, half * 512:(half + 1) * 512], ALPHA, px[:], ALU.mult, ALU.add),
                         r=[xr_, px], w=[hp])
                h1_ = h1[0]
                h1b_ = h1b[0]
                yield from ln_rows(hp, h1_, R("ln1_g"), R("ln1_b"), st4)
                S.dma("sp", ("h1st", 0), lambda q: q.dma_start(out=h1_t[ti], in_=h1_[:]), r=[h1_], w=[("h1_d", ti)])
                S.op("pool", lambda: P.tensor_copy(h1b_[:], h1_[:]), r=[h1_], w=[h1b_])
                if ti == 0:
                    dump("h1_0", h1_, h1_[:], [128, D])
                stage(13)
                yield
                for half in range(2):
                    pt = ps_q()
                    for qd in range(4):
                        kc = half * 4 + qd
                        S.op("pe", lambda: PE.transpose(pt[:, qd * 128:(qd + 1) * 128], h1_[:, kc * 128:(kc + 1) * 128], identf), r=[h1_, cst], w=[pt])
                    yield
                    S.op("act", lambda: A.activation(out=h1T[:, half * 4:half * 4 + 4, :], in_=v3(pt), func=AF.Copy), r=[pt], w=[h1T])
                pl = ps_q()
                for kc in range(8):
                    S.op("pe", lambda: PE.matmul(pl[:, 0:72], h1T[:, kc, :], wrt[:, kc, :], start=(kc == 0), stop=(kc == 7)), r=[h1T, wrt], w=[pl])
                rk = [rt]
                LG = rt[:, 0:72]
                yield
                S.op("dve", lambda: V.tensor_tensor(LG, pl[:, 0:72], R("br"), ALU.add), r=[pl, rowp], w=rk)
                S.op("dve", lambda: V.max(rt[:, 72:80], rt[:, 0:8]), r=rk, w=rk)
                S.op("dve", lambda: V.tensor_scalar(rt[:, 80:88], rt[:, 0:8], rt[:, 72:73], None, ALU.is_equal), r=rk, w=rk)
                yield
                S.op("dve", lambda: V.tensor_scalar(rt[:, 98:99], rt[:, 72:73], -1.0, None, ALU.mult), r=rk, w=rk)
                S.op("act", lambda: A.activation(out=rt[:, 88:96], in_=rt[:, 0:8], func=AF.Exp, bias=rt[:, 98:99], scale=1.0), r=rk, w=rk)
                S.op("dve", lambda: V.tensor_reduce(rt[:, 96:97], rt[:, 88:96], AX.X, ALU.add), r=rk, w=rk)
                yield
                S.op("dve", lambda: V.reciprocal(rt[:, 97:98], rt[:, 96:97]), r=rk, w=rk)
                el3 = rt[:, 8:72].rearrange("p (g j) -> p g j", g=8)
                tm3 = rt[:, 100:164].rearrange("p (g j) -> p g j", g=8)
                S.op("dve", lambda: V.tensor_tensor(tm3, el3, rt[:, 80:88].unsqueeze(2).broadcast_to([128, 8, 8]), ALU.mult), r=rk, w=rk)
                S.op("dve", lambda: V.tensor_reduce(rt[:, 164:172], rt[:, 100:164].rearrange("p (g j) -> p j g", g=8), AX.X, ALU.add), r=rk, w=rk)
                yield
                S.op("dve", lambda: V.max(rt[:, 172:180], rt[:, 164:172]), r=rk, w=rk)
                S.op("dve", lambda: V.tensor_scalar(rt[:, 180:188], rt[:, 164:172], rt[:, 172:173], None, ALU.is_equal), r=rk, w=rk)
                S.op("dve", lambda: V.tensor_scalar(rt[:, 188:196], rt[:, 164:172], rt[:, 173:174], None, ALU.is_equal), r=rk, w=rk)
                yield
                S.op("dve", lambda: V.tensor_tensor(rt[:, 196:197], rt[:, 173:174], rt[:, 172:173], ALU.subtract), r=rk, w=rk)
                S.op("act", lambda: A.activation(out=rt[:, 197:198], in_=rt[:, 196:197], func=AF.Exp), r=rk, w=rk)
                S.op("dve", lambda: V.tensor_scalar(rt[:, 198:199], rt[:, 197:198], 1.0, None, ALU.add), r=rk, w=rk)
                yield
                S.op("dve", lambda: V.reciprocal(rt[:, 198:199], rt[:, 198:199]), r=rk, w=rk)
                S.op("dve", lambda: V.tensor_tensor(rt[:, 199:200], rt[:, 197:198], rt[:, 198:199], ALU.mult), r=rk, w=rk)
                S.op("dve", lambda: V.tensor_scalar(RI[:, ti, 2:4], rt[:, 198:200], rt[:, 97:98], None, ALU.mult), r=rk, w=[RI])
                oh0 = rt[:, 200:264].rearrange("p (g j) -> p g j", g=8)
                oh1 = rt[:, 264:328].rearrange("p (g j) -> p g j", g=8)
                gohb = rt[:, 80:88].unsqueeze(2).broadcast_to([128, 8, 8])
                yield
                S.op("dve", lambda: V.tensor_tensor(oh0, gohb, rt[:, 180:188].unsqueeze(1).broadcast_to([128, 8, 8]), ALU.mult), r=rk, w=rk)
                S.op("dve", lambda: V.tensor_tensor(oh1, gohb, rt[:, 188:196].unsqueeze(1).broadcast_to([128, 8, 8]), ALU.mult), r=rk, w=rk)
                S.op("dve", lambda: V.tensor_tensor(OHb[:], rt[:, 200:264], rt[:, 264:328], ALU.add), r=rk, w=[OHb])
                pp = ps_q()
                S.op("pe", lambda: PE.matmul(pp[:, 0:64], sltrib, OHb[:], start=True, stop=True), r=[cb, OHb], w=[pp])
                S.op("pe", lambda: PE.matmul(pp[:, 64:128], onesb, OHb[:], start=True, stop=True), r=[cb, OHb], w=[pp])
                yield
                S.op("dve", lambda: V.tensor_tensor(rt[:, 328:392], pp[:, 0:64], carry[:], ALU.add), r=[pp, carry], w=rk)
                S.op("dve", lambda: V.tensor_tensor(carry[:], carry[:], pp[:, 64:128], ALU.add), r=[pp, carry], w=[carry])
                for k2 in range(2):
                    ohk = rt[:, 200 + 64 * k2:264 + 64 * k2]
                    S.op("dve", lambda: V.tensor_tensor(rt[:, 392:456], rt[:, 328:392], ohk, ALU.mult), r=rk, w=rk)
                    yield
                    S.op("dve", lambda: V.tensor_reduce(rt[:, 456 + k2:457 + k2], rt[:, 392:456], AX.X, ALU.add), r=rk, w=rk)
                    S.op("dve", lambda: V.tensor_tensor(rt[:, 392:456], R("iota"), ohk, ALU.mult), r=[rt, rowp], w=rk)
                    S.op("dve", lambda: V.tensor_reduce(rt[:, 458 + k2:459 + k2], rt[:, 392:456], AX.X, ALU.add), r=rk, w=rk)
                stage(14)
                yield
                S.op("dve", lambda: V.scalar_tensor_tensor(rt[:, 462:464], rt[:, 458:460], float(CAP), rt[:, 456:458], ALU.mult, ALU.add), r=rk, w=rk)
                S.op("dve", lambda: V.tensor_scalar(rt[:, 460:462], rt[:, 456:458], float(CAP), None, ALU.is_ge), r=rk, w=rk)
                S.op("dve", lambda: V.tensor_scalar(rt[:, 464:466], rt[:, 462:464], -1.0, float(TRASH), ALU.mult, ALU.add), r=rk, w=rk)
                yield
                S.op("dve", lambda: V.tensor_tensor(rt[:, 464:466], rt[:, 464:466], rt[:, 460:462], ALU.mult), r=rk, w=rk)
                S.op("dve", lambda: V.tensor_tensor(rt[:, 462:464], rt[:, 462:464], rt[:, 464:466], ALU.add), r=rk, w=rk)
                S.op("dve", lambda: V.tensor_scalar(RI[:, ti, 0:2], rt[:, 462:464], float(TRASH), 0.0, ALU.min, ALU.max), r=rk, w=[RI])
                S.op("dve", lambda: V.tensor_copy(RIi[:, ti, :], RI[:, ti, 0:2]), r=[RI], w=[RIi])
                stage(15)
                yield
                for k2 in range(2):
                    S.dma("pool", ("sc", k2), lambda q, k2=k2: q.indirect_dma_start(
                        out=xs_d, out_offset=bass.IndirectOffsetOnAxis(ap=RIi[:, ti, k2:k2 + 1], axis=0),
                        in_=h1b_[:], in_offset=None),
                        r=[h1b_, RIi] + [("xs_zero_last", i) for i in range(4)], w=[("xs_sc", ti, k2)])
                if ti == 0:
                    dump("rt0", rt, rt[:], [128, 512])

        n_tiles = n_blocks * TPB
        blk_done = [False] * (n_blocks + 2)
        q_done = [False] * (n_tiles + 2 * TPB)
        bgen = block_gen(0)
        b_cur = 0
        for _ in bgen:
            pass
        blk_done[0] = True
        bgen = None
        b_next = 1
        ps_gen, ps_tile_i = None, -1
        next_tile = 0
        q_queue = []
        ot_pending = False
        while True:
            progressed = False
            if ps_gen is None and next_tile < n_tiles and blk_done[next_tile // TPB] and not ot_pending:
                ps_gen, ps_tile_i = tile_gen(next_tile // TPB, next_tile % TPB), next_tile
                next_tile += 1
            if ps_gen is not None:
                progressed = True
                r = next(ps_gen)
                if r == "Q":
                    q_queue.append([ps_tile_i, ps_gen, 0])
                    ps_gen = None
                    ot_pending = True
            if q_queue:
                progressed = True
                ent = q_queue[0]
                try:
                    next(ent[1])
                    ent[2] += 1
                    if ent[2] == 1:
                        ot_pending = False
                except StopIteration:
                    q_done[ent[0]] = True
                    q_queue.pop(0)
                    if ent[2] == 0:
                        ot_pending = False
            if bgen is None and b_next < n_blocks and (b_next < 2 or q_done[(b_next - 2) * TPB + TPB - 1]):
                bgen, b_cur = block_gen(b_next), b_next
                b_next += 1
            if bgen is not None:
                progressed = True
                for _ in range(BLOCK_STEPS_PER_TILE_STEP):
                    try:
                        next(bgen)
                    except StopIteration:
                        blk_done[b_cur] = True
                        bgen = None
                        break
            if not progressed:
                break
        assert next_tile == n_tiles and not q_queue and ps_gen is None

        dump("RI", RI, RI[:], [128, NT, 4])

        S.barrier()
        es1.close()
        if stop_after == 1:
            raise _Cut()

        es2 = es.enter_context(ExitStack())
        NW = 3
        wgs = [sb(es2, "wg%d" % i, [128, 8, 512], BF16) for i in range(NW)]
        wus = [sb(es2, "wu%d" % i, [128, 8, 512], BF16) for i in range(NW)]
        wds = [sb(es2, "wd%d" % i, [128, 4, D], BF16) for i in range(NW)]
        Xe = [sb(es2, "Xe%d" % i, [128, RT, D], BF16) for i in range(NW)]
        xTe = [sb(es2, "xTe%d" % i, [128, 8, CAP], BF16) for i in range(NW)]
        sg = [sb(es2, "sg%d" % i, [128, CAP]) for i in range(2)]
        hid = [sb(es2, "hid%d" % i, [128, 4, CAP], BF16) for i in range(NW)]
        ysb = [sb(es2, "ysb%d" % i, [128, RT, D], BF16) for i in range(NW)]
        xs_e = xs_d[0:NE * CAP, :].rearrange("(e r p) d -> e p r d", p=128, r=RT)
        ys_e = ys_d[0:NE * CAP, :].rearrange("(e r p) d -> e p r d", p=128, r=RT)

        def load_expert(e):
            sl = e % NW
            S.dma("pool", ("wg", sl), lambda q: q.dma_start(out=wgs[sl][:], in_=wg_d[e].rearrange("(kc p) f -> p kc f", p=128)), w=[wgs[sl]])
            S.dma("pool", ("wu", sl), lambda q: q.dma_start(out=wus[sl][:], in_=wu_d[e].rearrange("(kc p) f -> p kc f", p=128)), w=[wus[sl]])
            S.dma("pool", ("wd", sl), lambda q: q.dma_start(out=wds[sl][:], in_=wd_d[e].rearrange("(fc p) d -> p fc d", p=128)), w=[wds[sl]])
            S.dma("sp", ("xe", e % NW), lambda q: q.dma_start(out=Xe[e % NW][:], in_=xs_e[e]), w=[Xe[e % NW]])

        for e in range(min(NW, n_experts)):
            load_expert(e)
        evac_i = [0]

        def evac(dst_ap, src_ap, rbuf, wbuf):
            if evac_i[0] % 2 == 0:
                S.op("act", lambda: A.activation(out=dst_ap, in_=src_ap, func=AF.Copy), r=[rbuf], w=[wbuf])
            else:
                S.op("dve", lambda: V.tensor_copy(dst_ap, src_ap), r=[rbuf], w=[wbuf])
            evac_i[0] += 1

        for e in range(n_experts):
            sl = e % NW
            X_ = Xe[e % NW]
            xT_ = xTe[e % NW]
            hid_ = hid[e % NW]
            ys_ = ysb[e % NW]
            for r_ in range(RT):
                pt = ps_next()
                pt_b = pt[:].bitcast(BF16)
                for kc in range(8):
                    S.op("pe", lambda: PE.transpose(pt_b[:, kc * 128:(kc + 1) * 128], X_[:, r_, kc * 128:(kc + 1) * 128], identb), r=[X_, cb], w=[pt])
                evac(xT_[:, :, r_ * 128:(r_ + 1) * 128], pt_b.rearrange("p (k j) -> p k j", k=8), pt, xT_)
            for fc in range(4):
                pg = ps_next()
                for kc in range(8):
                    S.op("pe", lambda: PE.matmul(pg[:, 0:CAP], wgs[sl][:, kc, fc * 128:(fc + 1) * 128], xT_[:, kc, :], start=(kc == 0), stop=(kc == 7)),
                         r=[wgs[sl], xT_], w=[pg])
                pu_ = ps_next()
                for kc in range(8):
                    S.op("pe", lambda: PE.matmul(pu_[:, 0:CAP], wus[sl][:, kc, fc * 128:(fc + 1) * 128], xT_[:, kc, :], start=(kc == 0), stop=(kc == 7)),
                         r=[wus[sl], xT_], w=[pu_])
                sg_ = sg[fc % 2]
                S.op("act", lambda: A.activation(out=sg_[:], in_=pg[:, 0:CAP], func=AF.Silu), r=[pg], w=[sg_])
                S.op("dve", lambda: V.tensor_tensor(hid_[:, fc, :], sg_[:], pu_[:, 0:CAP], ALU.mult), r=[sg_, pu_], w=[hid_])
            for r_ in range(RT):
                for dh in range(2):
                    py = ps_next()
                    for fc in range(4):
                        S.op("pe", lambda: PE.matmul(py[:], hid_[:, fc, r_ * 128:(r_ + 1) * 128], wds[sl][:, fc, dh * 512:(dh + 1) * 512], start=(fc == 0), stop=(fc == 3)),
                             r=[hid_, wds[sl]], w=[py])
                    evac(ys_[:, r_, dh * 512:(dh + 1) * 512], py[:], py, ys_)
            S.dma("sp", ("yst", e % NW), lambda q: q.dma_start(out=ys_e[e], in_=ys_[:]), r=[ys_], w=[("ys_e", e)])
            if e + NW < n_experts:
                load_expert(e + NW)
        S.barrier()
        es2.close()
        if stop_after == 2:
            raise _Cut()

        es3 = es.enter_context(ExitStack())
        NS3 = 4
        y0 = [sb(es3, "y0_%d" % i, [128, D], BF16) for i in range(NS3)]
        y1 = [sb(es3, "y1_%d" % i, [128, D], BF16) for i in range(NS3)]
        h1r = [sb(es3, "h1r%d" % i, [128, D]) for i in range(NS3)]
        accs = [sb(es3, "acc%d" % i, [128, D]) for i in range(NS3)]
        ob = [sb(es3, "ob%d" % i, [128, D]) for i in range(NS3)]
        junk = sb(es3, "junk3", [128, D])
        st3s = [sb(es3, "st3_%d" % i, [128, 16]) for i in range(NS3)]
        rowp2 = sb(es3, "rowp2", [128, 2 * D])
        S.dma("sp", "ld_rowp2", lambda q: q.dma_start(out=rowp2[:], in_=rowp2_d), w=[rowp2])
        for ti in range(NT):
            a0, a1, hr, o_ = y0[ti % NS3], y1[ti % NS3], h1r[ti % NS3], ob[ti % NS3]
            acc, st3 = accs[ti % NS3], st3s[ti % NS3]
            S.dma("pool", ("g0", ti % NS3), lambda q: q.indirect_dma_start(
                out=a0[:], out_offset=None, in_=ys_d, in_offset=bass.IndirectOffsetOnAxis(ap=RIi[:, ti, 0:1], axis=0)), r=[RIi], w=[a0])
            S.dma("pool", ("g1", ti % NS3), lambda q: q.indirect_dma_start(
                out=a1[:], out_offset=None, in_=ys_d, in_offset=bass.IndirectOffsetOnAxis(ap=RIi[:, ti, 1:2], axis=0)), r=[RIi], w=[a1])
            S.dma("sp", ("h1r", ti % NS3), lambda q: q.dma_start(out=hr[:], in_=h1_t[ti]), w=[hr])
            S.op("act", lambda: A.activation(out=acc[:], in_=a0[:], func=AF.Identity, scale=RI[:, ti, 2:3]), r=[a0, RI], w=[acc])
            S.op("dve", lambda: V.scalar_tensor_tensor(acc[:], a1[:], RI[:, ti, 3:4], acc[:], ALU.mult, ALU.add), r=[a1, RI, acc], w=[acc])
            S.op("dve", lambda: V.scalar_tensor_tensor(acc[:], hr[:], ALPHA, acc[:], ALU.mult, ALU.add), r=[hr, acc], w=[acc])
            for _ in ln_rows(acc, o_, rowp2[:, 0:D], rowp2[:, D:2 * D], st3, gbuf=rowp2):
                pass
            S.dma("sp", ("ost", ti % NS3), lambda q: q.dma_start(out=out_t[ti], in_=o_[:]), r=[o_], w=[("out", ti)])
        raise _Cut()


def host_inputs(inputs, b):
    f = np.float32
    g = lambda n: np.asarray(inputs[n], dtype=f)[0]
    rowp = np.zeros((NRP,), f)

    def put(n, v):
        o, w = RP[n]
        rowp[o:o + w] = np.asarray(v, f).reshape(-1)
    put("a_log", g("a_log")); put("dt_bias", g("dt_bias"))
    put("sgu_g", g("sgu_ln_g")); put("sgu_b", g("sgu_ln_b"))
    put("bsp", g("b_spatial"))
    put("ln1_g", g("ln1_g")); put("ln1_b", g("ln1_b"))
    rowp2 = np.ascontiguousarray(np.broadcast_to(np.concatenate([g("ln2_g"), g("ln2_b")])[None, :], (128, 2 * D)))
    put("br", np.concatenate([g("b_router_group"), g("b_router_expert")]))
    put("iota", np.arange(64))
    rowp = np.ascontiguousarray(np.broadcast_to(rowp[None, :], (128, NRP)))
    colp = np.zeros((128, 4), f)
    colp[:, 0] = g("dn_norm_w")
    convw_t = np.ascontiguousarray(g("conv_w").T.reshape(12, 128, 4).transpose(1, 0, 2))
    wst = np.ascontiguousarray(g("w_spatial").transpose(2, 0, 1))
    wr = np.ascontiguousarray(np.concatenate([g("w_router_group"), g("w_router_expert")], axis=1))
    i = np.arange(128)
    same = (i[:, None] // 64) == (i[None, :] // 64)
    cst = np.zeros((128, NCS, 128), f)
    cst[:, CS["ident"], :] = np.eye(128)
    cst[:, CS["ones"], :] = 1.0
    cst[:, CS["tri"], :] = ((i[:, None] <= i[None, :]) & same)
    cst[:, CS["chs"], :] = same
    cst[:, CS["maskS"], :] = np.where((i[None, :] < i[:, None]) & same, 0.0, NEG)
    cst[:, CS["maskST"], :] = np.where((i[None, :] > i[:, None]) & same, 0.0, NEG)
    cst[:, CS["maskIT"], :] = np.where((i[None, :] >= i[:, None]) & same, 0.0, NEG)
    cst[:, CS["sltri"], :] = (i[:, None] < i[None, :])
    cst[:, CS["maskWS"], :] = (i[None, :] >= i[:, None])
    return {
        "x": np.ascontiguousarray(np.asarray(inputs["x"], f)[b]),
        "w_in": g("w_in"), "w_out": g("w_out"), "convw_t": convw_t, "rowp": rowp, "rowp2": rowp2, "colp": colp,
        "wst": wst, "wr": wr, "w_gate": g("w_gate"), "w_up": g("w_up"), "w_down": g("w_down"),
        "cst": cst,
    }


def kernel(**inputs):
    nc, _ = build_program()
    shared = host_inputs(inputs, 0)
    in_maps = []
    for b in range(8):
        m = dict(shared)
        m["x"] = np.ascontiguousarray(np.asarray(inputs["x"], np.float32)[b])
        in_maps.append(m)
    res = run_bass_kernel_spmd(nc, in_maps, core_ids=list(range(8)))
    return np.stack([np.asarray(r["out"], np.float32) for r in res.results], axis=0)
```

```python
import bisect
from contextlib import ExitStack

import numpy as np
import concourse.bass as bass
import concourse.mybir as mybir
from concourse.bass_utils import run_bass_kernel_spmd

F32 = mybir.dt.float32
BF16 = mybir.dt.bfloat16
I32 = mybir.dt.int32
AF = mybir.ActivationFunctionType
ALU = mybir.AluOpType
AX = mybir.AxisListType

SEQ = 4096
D = 1024
NT = SEQ // 128
IN_COLS = 3080
C_Z = 1536
C_SP = 2048
C_U = 2056
C_VS = 2568
NE = 64
CAP = 384
RT = CAP // 128
TRASH = NE * CAP
NROWS = NE * CAP + 128
ALPHA = 2.0 ** 0.25
LN_EPS = 1e-5
RMS_EPS = 1e-6
NEG = -30000.0
ZF = True
SAME_ENGINE_RAW = True
EAGER_INC = True
LIST_SCHED = True
BANK_SPLIT = (3, 4, 1)
STRICT_SAME_ENGINE = True
SAME_ENGINE_GAP = 1000000
TILE_STEPS_PER_BLOCK_STEP = 1
BLOCK_STEPS_PER_TILE_STEP = 2
TPB = 2
BW = TPB * 128
NBLK = NT // TPB

RP = {}
_o = 0
for _n, _w in [("a_log", 4), ("dt_bias", 4), ("sgu_g", 512), ("sgu_b", 512), ("bsp", 512),
               ("ln1_g", 1024), ("ln1_b", 1024),
               ("br", 72), ("iota", 64)]:
    RP[_n] = (_o, _w)
    _o += _w
NRP = _o
CS = {n: i for i, n in enumerate(["ident", "ones", "tri", "chs", "maskS", "maskST", "maskIT",
                                  "sltri", "maskWS"])}
NCS = len(CS)


import heapq
import types

MODE = {"m": "emit"}


def _free_elems(ap):
    sh = ap.shape
    n = 1
    for v in sh[1:]:
        n *= int(v)
    return n


LINT = None


def _lint_check(tag, r, w, keyf):
    rk = set(keyf(t) for t in r)
    wk = set(keyf(t) for t in w)
    for name, is_out in LINT:
        if name.startswith("psb"):
            tok = ("ps", int(name[3:]))
        elif name.startswith("sb_"):
            tok = name[3:]
        else:
            continue
        if is_out:
            if tok not in wk:
                print("LINT: %s writes %s without declaring it (w=%s)" % (tag, tok, sorted(map(str, wk))))
        elif tok not in rk and tok not in wk:
            print("LINT: %s reads %s without declaring it (r=%s w=%s)" % (tag, tok, sorted(map(str, rk)), sorted(map(str, wk))))
    del LINT[:]


class EngProxy:
    def __init__(self, real, kind):
        self._real = real
        self._kind = kind

    def __getattr__(self, name):
        real = getattr(self._real, name) if self._real is not None else None
        kind = self._kind

        def call(*a, **kw):
            if MODE["m"] == "emit":
                return real(*a, **kw)
            if LINT is not None:
                outs = [kw["out"]] if "out" in kw else list(a[:1])
                for v in list(a) + list(kw.values()):
                    ap_ = getattr(v, "ap", v) if v.__class__.__name__ == "IndirectOffsetOnAxis" else v
                    if hasattr(ap_, "tensor") and hasattr(ap_, "shape"):
                        LINT.append((str(getattr(ap_.tensor, "name", ap_.name)), any(v is o for o in outs)))
            out = kw.get("out", a[0] if a else None)
            n = _free_elems(out) if out is not None and hasattr(out, "shape") else 64
            if kind == "pe":
                src = a[1] if len(a) > 1 else kw.get("in_", kw.get("lhsT"))
                mult = 1.0
                try:
                    if src.dtype == F32:
                        mult = 4.0 if name == "matmul" else 2.0
                except Exception:
                    pass
                return 0.03 + n * 0.65e-3 * mult
            if kind == "act":
                return 0.2 + n * 0.95e-3
            if kind == "dve":
                return 0.12 + n * 1.05e-3
            if kind == "pool":
                return 0.15 + n * 1.9e-3
            tot = n * int(out.shape[0]) if out is not None and hasattr(out, "shape") else 1 << 16
            src = kw.get("in_", a[1] if len(a) > 1 else None)
            if src is not None and hasattr(src, "shape"):
                tot = min(tot, _free_elems(src) * int(src.shape[0]))
            return ("dma", 2.0 + tot * 3.0 / 150e3)
        return call


def _snapshot(fn):
    if fn.__closure__ is None:
        return fn
    cells = []
    for c in fn.__closure__:
        try:
            cells.append(types.CellType(c.cell_contents))
        except ValueError:
            cells.append(c)
    return types.FunctionType(fn.__code__, fn.__globals__, fn.__name__, fn.__defaults__, tuple(cells))


class Buf:
    def __init__(self, t, key):
        self.t = t
        self.k = key

    def __getitem__(self, idx):
        return self.t[idx]


class Sched:
    def __init__(self, nc, es):
        self.nc = nc
        self.es = es
        self.eng = {"pe": nc.tensor, "dve": nc.vector, "act": nc.scalar, "pool": nc.gpsimd,
                    "sp": nc.sync}
        self.sem = {k: es.enter_context(nc.semaphore("sem_" + k)) for k in ("pe", "dve", "act", "pool")}
        self.insts = {k: [] for k in self.sem}
        self.inc_idx = {k: [] for k in self.sem}
        self.inc_cnt = {k: [] for k in self.sem}
        self.seen = {k: {} for k in self.eng}
        self.dsem = {}
        self.last_w = {}
        self.readers = {}
        self.n_wait = 0
        self.log = None
        self.defer = LIST_SCHED
        self.rec = []
        self.rlast_w = {}
        self.rreaders = {}
        self.rkey_last = {}
        self.mq = EngProxy(None, "dmaq")
        self.pe_prev = None
        self.label = ""
        self.labels = {k: [] for k in self.sem}

    @staticmethod
    def _key(b):
        return b.k if isinstance(b, Buf) else b

    def _resolve(self, ref):
        if ref[0] == "e":
            _, eng, idx = ref
            ii = self.inc_idx[eng]
            p = bisect.bisect_left(ii, idx)
            if p < len(ii):
                return ("e", eng), self.sem[eng], self.inc_cnt[eng][p]
            cnt = (self.inc_cnt[eng][-1] if ii else 0) + 1
            self.insts[eng][idx].then_inc(self.sem[eng], 1)
            if self.log is not None:
                self.log.append("   inc %s[%d] -> %d" % (eng, idx, cnt))
            ii.append(idx)
            self.inc_cnt[eng].append(cnt)
            return ("e", eng), self.sem[eng], cnt
        _, key, cnt = ref
        return ("d", key), self.dsem[key][0], cnt

    def _wait(self, consumer, ref, raw=False):
        if ref[0] == "e" and ref[1] == consumer:
            if consumer == "pe" or not SAME_ENGINE_RAW:
                return
            if not raw and not STRICT_SAME_ENGINE:
                return
            if len(self.insts[consumer]) - ref[2] > SAME_ENGINE_GAP:
                return
        name, sem, cnt = self._resolve(ref)
        if self.seen[consumer].get(name, 0) >= cnt:
            return
        self.eng[consumer].wait_ge(sem, cnt)
        if self.log is not None:
            self.log.append("   %s waits %s >= %d" % (consumer, name, cnt))
        self.seen[consumer][name] = cnt
        self.n_wait += 1

    def _deps(self, consumer, reads, writes):
        for t in reads:
            k = self._key(t)
            if k in self.last_w:
                self._wait(consumer, self.last_w[k], raw=True)
        for t in writes:
            k = self._key(t)
            if k in self.last_w:
                self._wait(consumer, self.last_w[k])
            for r in self.readers.get(k, ()):
                self._wait(consumer, r)

    def _record(self, ref, reads, writes):
        for t in reads:
            k = self._key(t)
            lst = self.readers.setdefault(k, [])
            src = ref[:2]
            lst[:] = [r for r in lst if r[:2] != src]
            lst.append(ref)
        for t in writes:
            k = self._key(t)
            self.last_w[k] = ref
            self.readers[k] = []

    def _psx(self, r, w):
        ps = [t for t in r if isinstance(self._key(t), tuple) and self._key(t)[0] == "ps"]
        if not ps:
            return r, w
        return [t for t in r if t not in ps], list(w) + [t for t in ps if t not in w]

    def _rec_deps(self, r, w):
        deps = set()
        for t in r:
            k = self._key(t)
            if k in self.rlast_w:
                deps.add(self.rlast_w[k])
        for t in w:
            k = self._key(t)
            if k in self.rlast_w:
                deps.add(self.rlast_w[k])
            deps.update(self.rreaders.get(k, ()))
        return deps

    def _rec_note(self, i, r, w):
        for t in r:
            self.rreaders.setdefault(self._key(t), []).append(i)
        for t in w:
            k = self._key(t)
            self.rlast_w[k] = i
            self.rreaders[k] = []

    def op(self, eng, fn, r=(), w=()):
        if not self.defer:
            return self._emit_op(eng, fn, r, w)
        r2, w2 = self._psx(r, w)
        fn = _snapshot(fn)
        MODE["m"] = "measure"
        try:
            cost = fn()
        finally:
            MODE["m"] = "emit"
        if LINT is not None:
            _lint_check("op %s #%d [%s]" % (eng, len(self.rec), self.label), r, w, self._key)
        i = len(self.rec)
        deps = self._rec_deps(r2, w2)
        self.rec.append(("op", eng, fn, r, w, float(cost), deps, self.label))
        self._rec_note(i, r2, w2)

    def dma(self, q, key, fn, r=(), w=()):
        if not self.defer:
            return self._emit_dma(q, key, fn, r, w)
        fn = _snapshot(fn)
        MODE["m"] = "measure"
        try:
            cost = fn(self.mq)
        finally:
            MODE["m"] = "emit"
        lat = cost[1] if isinstance(cost, tuple) else 3.0
        if LINT is not None:
            _lint_check("dma %s key=%s" % (q, key), r, w, self._key)
        i = len(self.rec)
        deps = self._rec_deps(r, w)
        kdep = self.rkey_last.get(key, -1)
        self.rkey_last[key] = i
        self.rec.append(("dma", q, fn, r, w, lat, deps, key, kdep))
        self._rec_note(i, r, w)

    def flush(self):
        rec = self.rec
        n = len(rec)
        if n == 0:
            return
        succ = [[] for _ in range(n)]
        ksucc = [-1] * n
        ndep = [0] * n
        for i, e in enumerate(rec):
            ndep[i] = len(e[6])
            for d in e[6]:
                succ[d].append(i)
            if e[0] == "dma" and e[8] >= 0 and e[8] not in e[6]:
                ksucc[e[8]] = i
                ndep[i] += 1
        blev = [0.0] * n
        for i in range(n - 1, -1, -1):
            e = rec[i]
            b = 0.0
            for j in succ[i]:
                if blev[j] > b:
                    b = blev[j]
            blev[i] = b + e[5] + 0.3
        ready_t = [0.0] * n
        finish = [0.0] * n
        eng_free = {}
        engs = {}
        for i in range(n):
            engs.setdefault(rec[i][1], [[], []])
        for i in range(n):
            if ndep[i] == 0:
                heapq.heappush(engs[rec[i][1]][0], (0.0, i))
        order = []
        t_base = 0.0
        n_done = 0
        while n_done < n:
            best = None
            for eng, (fut, rdy) in engs.items():
                free = eng_free.get(eng, t_base)
                while fut and fut[0][0] <= free:
                    rt_, j = heapq.heappop(fut)
                    heapq.heappush(rdy, (-blev[j], j))
                if rdy:
                    cand = (free, eng)
                elif fut:
                    cand = (fut[0][0], eng)
                else:
                    continue
                if best is None or cand < best:
                    best = cand
            tstart, eng = best
            fut, rdy = engs[eng]
            if not rdy:
                while fut and fut[0][0] <= tstart:
                    rt_, j = heapq.heappop(fut)
                    heapq.heappush(rdy, (-blev[j], j))
            _, i = heapq.heappop(rdy)
            n_done += 1
            e = rec[i]
            est = ready_t[i]
            start = max(est, eng_free.get(eng, t_base))
            if e[0] == "op":
                fin = start + e[5]
                eng_free[eng] = fin
            else:
                issue = 0.15 if eng == "sp" else 0.8
                eng_free[eng] = start + issue
                fin = start + e[5]
            finish[i] = fin
            order.append(i)
            for j in succ[i]:
                lat = 0.05 if rec[j][1] == eng and e[0] == "op" else 0.3
                ready_t[j] = max(ready_t[j], fin + lat)
                ndep[j] -= 1
                if ndep[j] == 0:
                    heapq.heappush(engs[rec[j][1]][0], (ready_t[j], j))
            j = ksucc[i]
            if j >= 0:
                ready_t[j] = max(ready_t[j], eng_free[eng])
                ndep[j] -= 1
                if ndep[j] == 0:
                    heapq.heappush(engs[rec[j][1]][0], (ready_t[j], j))
        assert len(order) == n
        self.sim_time = getattr(self, "sim_time", 0.0) + max(finish)
        self.rec = []
        self.rlast_w = {}
        self.rreaders = {}
        self.rkey_last = {}
        for i in order:
            e = rec[i]
            if e[0] == "op":
                self.label = e[7]
                self._emit_op(e[1], e[2], e[3], e[4])
            else:
                self._emit_dma(e[1], e[7], e[2], e[3], e[4])

    def _emit_op(self, eng, fn, r=(), w=()):
        r, w = self._psx(r, w)
        self._deps(eng, r, w)
        inst = fn()
        idx = len(self.insts[eng])
        self.insts[eng].append(inst)
        self.labels[eng].append(self.label)
        if EAGER_INC:
            if eng != "pe":
                self._resolve(("e", eng, idx))
            else:
                wk = tuple(self._key(t) for t in w)
                if self.pe_prev is not None and self.pe_prev[1] != wk:
                    self._resolve(("e", "pe", self.pe_prev[0]))
                self.pe_prev = (idx, wk)
        if self.log is not None:
            self.log.append("%s[%d] r=%s w=%s" % (eng, idx, [self._key(t) for t in r], [self._key(t) for t in w]))
        ref = ("e", eng, idx)
        self._record(ref, r, w)
        return ref

    def _emit_dma(self, q, key, fn, r=(), w=()):
        self._deps(q, r, w)
        if key not in self.dsem:
            self.dsem[key] = [self.es.enter_context(self.nc.semaphore("dq%d" % len(self.dsem))), 0]
        ent = self.dsem[key]
        inst = fn(self.eng[q])
        inst.then_inc(ent[0], 16)
        ent[1] += 16
        if self.log is not None:
            self.log.append("dma on %s key=%s -> %d r=%s w=%s" % (q, key, ent[1], [self._key(t) for t in r], [self._key(t) for t in w]))
        ref = ("d", key, ent[1])
        self._record(ref, r, w)
        return ref

    def barrier(self, engines=("pe", "dve", "act", "pool", "sp")):
        self.flush()
        for c in engines:
            for e in self.sem:
                if e != c and self.insts[e]:
                    self._wait(c, ("e", e, len(self.insts[e]) - 1))
            for key, ent in self.dsem.items():
                if ent[1]:
                    self._wait(c, ("d", key, ent[1]))
        self.last_w.clear()
        self.readers.clear()

    def final_wait(self, q="sp"):
        self.flush()
        for key, ent in self.dsem.items():
            if ent[1]:
                self._wait(q, ("d", key, ent[1]))


class _Cut(Exception):
    pass


def build_program(dbg=None, stop_after=None, n_blocks=NBLK, n_experts=NE, cut=None):
    nc = bass.Bass("TRN2", target_bir_lowering=False)
    dt_in = lambda n, s: nc.dram_tensor(n, s, F32, kind="ExternalInput").ap()
    x_d = dt_in("x", [SEQ, D])
    w_in_d = dt_in("w_in", [D, IN_COLS])
    w_out_d = dt_in("w_out", [D, D])
    convw_d = dt_in("convw_t", [128, 12, 4])
    rowp_d = dt_in("rowp", [128, NRP])
    rowp2_d = dt_in("rowp2", [128, 2 * D])
    colp_d = dt_in("colp", [128, 4])
    wst_d = dt_in("wst", [128, 4, 128])
    wr_d = dt_in("wr", [D, 72])
    wg_d = dt_in("w_gate", [NE, D, 512])
    wu_d = dt_in("w_up", [NE, D, 512])
    wd_d = dt_in("w_down", [NE, 512, D])
    cst_d = dt_in("cst", [128, NCS, 128])
    out_d = nc.dram_tensor("out", [SEQ, D], F32, kind="ExternalOutput").ap()
    h1_d = nc.dram_tensor("h1_scr", [SEQ, D], F32).ap()
    xs_d = nc.dram_tensor("xs_scr", [NROWS, D], BF16).ap()
    ys_d = nc.dram_tensor("ys_scr", [NROWS, D], BF16).ap()

    dbg_outs = {}

    def stage(n):
        if cut is not None and n == cut:
            raise _Cut()
        if n != 3 and not (20 <= n < 40):
            SCHED[0].label = "s%d" % n
        else:
            SCHED[0].label = "blk"

    SCHED = [None]

    with ExitStack() as es:
        S = Sched(nc, es)
        SCHED[0] = S
        if dbg is not None and "LABELS" in dbg:
            dbg["LABELS"] = S.labels
        if dbg is not None and "LOG" in dbg:
            S.log = dbg["LOG"]
        try:
            _emit(nc, es, S, stage, dbg, dbg_outs, stop_after, n_blocks, n_experts, locals())
        except _Cut:
            S.final_wait()
    return nc, dbg_outs


def _emit(nc, es, S, stage, dbg, dbg_outs, stop_after, n_blocks, n_experts, env):
    x_d, w_in_d, w_out_d, convw_d, rowp_d, rowp2_d, colp_d, wst_d, wr_d, wg_d, wu_d, wd_d, cst_d, out_d, h1_d, xs_d, ys_d = (
        env[k] for k in "x_d w_in_d w_out_d convw_d rowp_d rowp2_d colp_d wst_d wr_d wg_d wu_d wd_d cst_d out_d h1_d xs_d ys_d".split())
    if True:
        V, A, P, PE = EngProxy(nc.vector, "dve"), EngProxy(nc.scalar, "act"), EngProxy(nc.gpsimd, "pool"), EngProxy(nc.tensor, "pe")

        def sb(es_, name, shape, dt=F32):
            return Buf(es_.enter_context(nc.sbuf_tensor("sb_" + name, shape, dt)), name)

        banks = [Buf(es.enter_context(nc.psum_tensor("psb%d" % i, [128, 512], F32)), ("ps", i))
                 for i in range(8)]
        bank_i = [0]

        def ps_next():
            b = banks[bank_i[0] % 8]
            bank_i[0] += 1
            return b

        bankb_i = [0]
        bankt_i = [0]

        bankq_i = [0]
        NB_B, NB_T, NB_Q = BANK_SPLIT

        def ps_blk():
            b = banks[bankb_i[0] % NB_B]
            bankb_i[0] += 1
            return b

        def ps_tile():
            b = banks[NB_B + bankt_i[0] % (NB_T - 1)]
            bankt_i[0] += 1
            return b

        def ps_acc():
            return banks[NB_B + NB_T - 1]

        def ps_q():
            b = banks[NB_B + NB_T + bankq_i[0] % NB_Q]
            bankq_i[0] += 1
            return b

        def dump(name, buf, ap, shape, dt=F32):
            if dbg is None or name not in dbg:
                return
            o = nc.dram_tensor("dbg_" + name, shape, dt, kind="ExternalOutput").ap()
            dbg_outs[name] = o
            S.dma("sp", ("dbg", name), lambda q: q.dma_start(out=o, in_=ap), r=[buf], w=[("dbgd", name)])

        cst = sb(es, "cst", [128, NCS, 128])
        rowp = sb(es, "rowp", [128, NRP])
        colp = sb(es, "colp", [128, 4])
        S.dma("sp", "ld_cst", lambda q: q.dma_start(out=cst[:], in_=cst_d), w=[cst])
        S.dma("sp", "ld_rowp", lambda q: q.dma_start(out=rowp[:], in_=rowp_d), w=[rowp])
        S.dma("sp", "ld_colp", lambda q: q.dma_start(out=colp[:], in_=colp_d), w=[colp])

        def C(n):
            return cst[:, CS[n], :]

        def R(n, lo=0, hi=None):
            o, wd = RP[n]
            hi = wd if hi is None else hi
            return rowp[:, o + lo:o + hi]

        cb = sb(es, "cstb", [128, 3, 128], BF16)
        S.op("dve", lambda: V.tensor_copy(cb[:, 0, :], C("ident")), r=[cst], w=[cb])
        S.op("dve", lambda: V.tensor_copy(cb[:, 1, :], C("ones")), r=[cst], w=[cb])
        S.op("dve", lambda: V.tensor_copy(cb[:, 2, :], C("sltri")), r=[cst], w=[cb])
        identb, onesb, sltrib = cb[:, 0, :], cb[:, 1, :], cb[:, 2, :]
        identf, onesf = C("ident"), C("ones")

        small = sb(es, "small", [128, 16])
        S.op("dve", lambda: V.memset(small[:, 4:5], RMS_EPS), w=[small])
        S.op("dve", lambda: V.memset(small[:, 5:6], LN_EPS), w=[small])
        S.op("dve", lambda: V.memset(small[:, 6:7], float(np.log(128.0 ** -0.5))), w=[small])
        S.op("dve", lambda: V.memset(small[:, 7:8], 1.0), w=[small])
        S.op("dve", lambda: V.memset(small[:, 8:10], 0.0), w=[small])
        S.op("dve", lambda: V.memset(small[0:64, 8:9], 1.0), w=[small])
        S.op("dve", lambda: V.memset(small[64:128, 9:10], 1.0), w=[small])
        S.op("act", lambda: A.activation(out=small[:, 0:4], in_=R("a_log"), func=AF.Exp), r=[rowp, small], w=[small])
        S.op("dve", lambda: V.tensor_scalar(small[:, 0:4], small[:, 0:4], -1.0, None, ALU.mult), r=[small], w=[small])
        EPS_RMS, EPS_LN, LNQS, ONE = small[:, 4:5], small[:, 5:6], small[:, 6:7], small[:, 7:8]
        CM = [small[:, 8:9], small[:, 9:10]]

        RI = sb(es, "RI", [128, NT, 4])
        RIi = sb(es, "RIi", [128, NT, 2], I32)

        es1 = es.enter_context(ExitStack())
        w_in_b = sb(es1, "w_in_b", [128, 8, IN_COLS], BF16)
        w_out_b = sb(es1, "w_out_b", [128, 8, D], BF16)
        wrt = sb(es1, "wrt", [128, 8, 72])
        convw = sb(es1, "convw", [128, 12, 4])
        cdiag = sb(es1, "cdiag", [128, 48, 128], BF16)
        wstb = sb(es1, "wstb", [128, 4, 128], BF16)
        w_in_v = w_in_d.rearrange("(kc p) c -> p kc c", p=128)
        for kc in range(8):
            S.dma("pool", ("ld_win", kc), lambda q, kc=kc: q.dma_start(out=w_in_b[:, kc, :], in_=w_in_v[:, kc, :]), w=[w_in_b])
        S.dma("sp", "ld_convw", lambda q: q.dma_start(out=convw[:], in_=convw_d), w=[convw])
        S.dma("pool", "ld_wst", lambda q: q.dma_start(out=wstb[:], in_=wst_d), w=[wstb])
        S.dma("sp", "ld_wr", lambda q: q.dma_start(out=wrt[:], in_=wr_d.rearrange("(kc p) c -> p kc c", p=128)), w=[wrt])
        w_out_v = w_out_d.rearrange("(kc p) c -> p kc c", p=128)
        for kc in range(0, 8, 4):
            S.dma("pool", ("ld_wout", kc), lambda q, kc=kc: q.dma_start(out=w_out_b[:, kc:kc + 4, :], in_=w_out_v[:, kc:kc + 4, :]), w=[w_out_b])
        for cc in range(12):
            for j in range(4):
                S.op("dve", lambda cc=cc, j=j: V.tensor_scalar(cdiag[:, cc * 4 + j, :], identf, convw[:, cc, j:j + 1], None, ALU.mult),
                     r=[cst, convw], w=[cdiag])
        S.op("dve", lambda: V.tensor_tensor(wstb[:], wstb[:], C("maskWS").unsqueeze(1).broadcast_to([128, 4, 128]), ALU.mult),
             r=[wstb, cst], w=[wstb])

        stage(1)
        xsl = [sb(es1, "xsl%d" % i, [128, D], BF16) for i in range(2)]
        xr = [sb(es1, "xr%d" % i, [128, D]) for i in range(1)]
        xTb = sb(es1, "xTb", [128, 8, BW], BF16)
        eB = [sb(es1, "eB%d" % i, [128, 512]) for i in range(2)]
        sp8 = [sb(es1, "sp8_%d" % i, [128, 8]) for i in range(2 * TPB)]
        raw = [sb(es1, "raw%d" % i, [128, BW + 3], BF16) for i in range(2)]
        halo = sb(es1, "halo", [128, 12, 3], BF16)
        qs = [sb(es1, "qs%d" % i, [128, BW]) for i in range(1)]
        sqb = [sb(es1, "sqb%d" % i, [128, 512], BF16) for i in range(1)]
        sqbB = [sb(es1, "sqbB%d" % i, [128, BW], BF16) for i in range(2)]
        tmpB = [sb(es1, "tmpB%d" % i, [128, BW]) for i in range(2)]
        qT2 = [sb(es1, "qT%d" % i, [128, 4, BW], BF16) for i in range(2)]
        kT2 = [sb(es1, "kT%d" % i, [128, 4, BW], BF16) for i in range(2)]
        vT2 = [sb(es1, "vT%d" % i, [128, 4, BW], BF16) for i in range(2)]
        ZG2 = [sb(es1, "ZG%d" % i, [128, 4, BW], BF16) for i in range(2)]
        UG2 = [sb(es1, "UG%d" % i, [128, 4, BW], BF16) for i in range(2)]
        YT = sb(es1, "YT", [128, 8, BW], BF16)
        XC = sb(es1, "XC", [128, 4, 128])
        VLN = [sb(es1, "VLN%d" % i, [128, 512], BF16) for i in range(2 * TPB)]
        st4 = sb(es1, "st4", [128, 16])
        st4B = sb(es1, "st4B", [128, 16])
        cf2 = [sb(es1, "cf%d" % i, [128, 64]) for i in range(2)]
        Dg = sb(es1, "Dg", [128, 4, 128])
        X1 = Dg
        X3 = sb(es1, "X3", [128, 4, 128])
        E1, E3 = X1, X3
        tQ1 = sb(es1, "tQ1", [128, 512])
        tQ2 = tQ1
        EG2 = [sb(es1, "EG%d" % i, [128, 4, 128]) for i in range(2)]
        Mb = [sb(es1, "Mb%d" % i, [128, 4, 128], BF16) for i in range(2)]
        MTb = [sb(es1, "MTb%d" % i, [128, 4, 128], BF16) for i in range(2)]
        Pb = [sb(es1, "Pb%d" % i, [128, 4, 128], BF16) for i in range(2)]
        AinT2 = [sb(es1, "AinT%d" % i, [128, 4, 128], BF16) for i in range(2)]
        QDT2 = [sb(es1, "QDT%d" % i, [128, 4, 128], BF16) for i in range(2)]
        RHSw = sb(es1, "RHSw", [128, 4, 128], BF16)
        RHSu = sb(es1, "RHSu", [128, 4, 128], BF16)
        KD2 = [[sb(es1, "KD%d_%d" % (j, i), [128, 4, 128], BF16) for i in range(2)] for j in range(2)]
        Uf2 = [sb(es1, "Uf%d" % i, [128, 4, 128]) for i in range(2)]
        WTb2 = [sb(es1, "WTb%d" % i, [128, 4, 128], BF16) for i in range(2)]
        VN = sb(es1, "VN", [128, 4, 128], BF16)
        Sf = sb(es1, "Sf", [128, 4, 128])
        Sb = sb(es1, "Sb", [128, 4, 128], BF16)
        OT = sb(es1, "OT", [128, 4, 128])
        hp = xr[0]
        h1 = [sb(es1, "h1_%d" % i, [128, D]) for i in range(1)]
        h1b = [sb(es1, "h1b_%d" % i, [128, D], BF16) for i in range(1)]
        h1T = sb(es1, "h1T", [128, 8, 128])
        junk = Buf(h1T.t[:].rearrange("p k j -> p (k j)"), h1T.k)
        rt = sb(es1, "rt", [128, 512])
        OHb = sb(es1, "OHb", [128, 64], BF16)
        carry = sb(es1, "carry", [128, 64])
        zero_b = Buf(rt.t[:].bitcast(BF16).rearrange("p (o d) -> p o d", o=1), rt.k)

        S.op("dve", lambda: V.memset(Sf[:], 0.0), w=[Sf])
        S.op("dve", lambda: V.memset(Sb[:], 0.0), w=[Sb])
        S.op("dve", lambda: V.memset(VN[:], 0.0), w=[VN])
        S.op("dve", lambda: V.memset(halo[:], 0.0), w=[halo])
        S.op("dve", lambda: V.memset(carry[:], 0.0), w=[carry])
        S.op("pool", lambda: P.memset(rt[:], 0.0), w=[rt])

        def ln_rows(src, dst, g_ap, b_ap, scr, gbuf=None):
            gbuf = rowp if gbuf is None else gbuf
            S.op("dve", lambda: V.tensor_reduce(scr[:, 0:1], src[:], AX.X, ALU.add), r=[src], w=[scr])
            yield
            S.op("dve", lambda: V.tensor_scalar(scr[:, 1:2], scr[:, 0:1], -1.0 / D, None, ALU.mult), r=[scr], w=[scr])
            yield
            S.op("act", lambda: A.activation(out=junk[:], in_=src[:], func=AF.Square, bias=scr[:, 1:2], scale=1.0,
                                             accum_out=scr[:, 2:3]), r=[src, scr], w=[junk, scr])
            yield
            S.op("act", lambda: A.activation(out=scr[:, 3:4], in_=scr[:, 2:3], func=AF.Ln, bias=EPS_LN, scale=1.0 / D),
                 r=[scr, small], w=[scr])
            yield
            S.op("act", lambda: A.activation(out=scr[:, 4:5], in_=scr[:, 3:4], func=AF.Exp, scale=-0.5), r=[scr], w=[scr])
            yield
            S.op("dve", lambda: V.tensor_tensor(scr[:, 5:6], scr[:, 1:2], scr[:, 4:5], ALU.mult), r=[scr], w=[scr])
            yield
            S.op("act", lambda: A.activation(out=dst[:], in_=src[:], func=AF.Identity, bias=scr[:, 5:6], scale=scr[:, 4:5]), r=[src, scr], w=[dst])
            yield
            S.op("pool", lambda: P.tensor_tensor(dst[:], dst[:], g_ap, ALU.mult), r=[dst, gbuf], w=[dst])
            yield
            S.op("dve", lambda: V.tensor_tensor(dst[:], dst[:], b_ap, ALU.add), r=[dst, gbuf], w=[dst])
            yield

        x_t = x_d.rearrange("(n p) d -> n p d", p=128)
        h1_t = h1_d.rearrange("(n p) d -> n p d", p=128)
        out_t = out_d.rearrange("(n p) d -> n p d", p=128)

        def bc_h(ap2):
            return ap2.unsqueeze(1).broadcast_to([128, 4, 128])

        def bc_j(ap2):
            return ap2.unsqueeze(2).broadcast_to([128, 4, 128])

        def v3(b):
            return b[:].rearrange("p (h j) -> p h j", h=4)

        def load_x(ti):
            S.dma("pool", ("x", ti % 2), lambda q: q.dma_start(out=xsl[ti % 2][:], in_=x_t[ti]), w=[xsl[ti % 2]])

        zero_done = [0]

        def zero_fill(n):
            zr = xs_d.rearrange("(n r p) d -> n p r d", p=128, r=1)
            tot = NROWS // 128
            for _ in range(n):
                i = zero_done[0]
                if i < tot:
                    wt = [("xs_zero", i)] + ([("xs_zero_last", i % 4)] if i >= tot - 4 + 1 else [])
                    S.dma("sp", ("zf", i % 4), lambda q, i=i: q.dma_start(out=zr[i], in_=zero_b[:]), r=[zero_b], w=wt)
                elif i == tot:
                    S.dma("sp", ("zf", i % 4), lambda q: q.dma_start(out=ys_d[TRASH:TRASH + 128, :], in_=zero_b[:, 0, :]), r=[zero_b], w=["ys_trash", ("xs_zero_last", i % 4)])
                zero_done[0] += 1
        assert NROWS % 128 == 0

        stage(2)
        for ti in range(2):
            load_x(ti)

        def act_sigmoid(dst_ap, dst_buf, src_ap, src_buf, scale=1.0):
            S.op("act", lambda: A.activation(out=dst_ap, in_=src_ap, func=AF.Exp, scale=-scale), r=[src_buf], w=[dst_buf])
            S.op("act", lambda: A.activation(out=dst_ap, in_=dst_ap, func=AF.Ln, bias=ONE, scale=1.0), r=[dst_buf, small], w=[dst_buf])
            S.op("act", lambda: A.activation(out=dst_ap, in_=dst_ap, func=AF.Exp, scale=-1.0), r=[dst_buf], w=[dst_buf])

        GC_A = float(np.sqrt(0.044715))
        GC_B = float(2.0 * np.sqrt(2.0 / np.pi))

        def gelu_tanh(dst_ap, dst_buf, src_ap, src_buf, e_ap, e_buf):
            S.op("act", lambda: A.activation(out=e_ap, in_=src_ap, func=AF.Square, scale=GC_A), r=[src_buf], w=[e_buf])
            S.op("dve", lambda: V.scalar_tensor_tensor(e_ap, e_ap, 1.0, src_ap, ALU.add, ALU.mult), r=[e_buf, src_buf], w=[e_buf])
            act_sigmoid(e_ap, e_buf, e_ap, e_buf, scale=GC_B)
            S.op("dve", lambda: V.tensor_tensor(dst_ap, src_ap, e_ap, ALU.mult), r=[src_buf, e_buf], w=[dst_buf])

        def block_gen(blk):
            bs = blk % 2
            qT, kT, vT, ZG, UG = qT2[bs], kT2[bs], vT2[bs], ZG2[bs], UG2[bs]
            for tl in range(TPB):
                ti = blk * TPB + tl
                xs_ = xsl[ti % 2]
                pb = ps_blk()
                pb_b = pb[:].bitcast(BF16)
                for kc in range(8):
                    S.op("pe", lambda: PE.transpose(pb_b[:, kc * 128:(kc + 1) * 128], xs_[:, kc * 128:(kc + 1) * 128], identb),
                         r=[xs_, cb], w=[pb])
                yield
                S.op("act", lambda: A.activation(out=xTb[:, :, tl * 128:(tl + 1) * 128], in_=pb_b.rearrange("p (k j) -> p k j", k=8), func=AF.Copy),
                     r=[pb], w=[xTb])
                if ti + 2 < n_blocks * TPB:
                    load_x(ti + 2)
                yield
                pb = ps_blk()
                for kc in range(8):
                    S.op("pe", lambda: PE.matmul(pb[:, 0:8], xTb[:, kc, tl * 128:(tl + 1) * 128], w_in_b[:, kc, C_SP:C_SP + 8], start=(kc == 0), stop=(kc == 7)),
                         r=[xTb, w_in_b], w=[pb])
                yield
                S.op("dve", lambda: V.tensor_copy(sp8[bs * TPB + tl][:], pb[:, 0:8]), r=[pb], w=[sp8[bs * TPB + tl]])
                yield
            if blk == 0 and ZF:
                zero_fill(NROWS // 128 + 1)

            stage(3)
            yield
            def proj_chunk(col0):
                pb_ = ps_blk()
                for kc in range(8):
                    S.op("pe", lambda: PE.matmul(pb_[:, 0:BW], w_in_b[:, kc, col0:col0 + 128], xTb[:, kc, :], start=(kc == 0), stop=(kc == 7)),
                         r=[w_in_b, xTb], w=[pb_])
                return pb_

            for cc in range(12):
                pb = proj_chunk(cc * 128)
                yield
                rw = raw[cc % 2]
                S.op("pool", lambda: P.tensor_copy(rw[:, 0:3], halo[:, cc, :]), r=[halo], w=[rw])
                S.op("act", lambda: A.activation(out=rw[:, 3:BW + 3], in_=pb[:, 0:BW], func=AF.Copy), r=[pb], w=[rw])
                S.op("pool", lambda: P.tensor_copy(halo[:, cc, :], rw[:, BW:BW + 3]), r=[rw], w=[halo])
                pc = ps_blk()
                for j in range(4):
                    S.op("pe", lambda: PE.matmul(pc[:, 0:BW], cdiag[:, cc * 4 + j, :], rw[:, j:j + BW], start=(j == 0), stop=(j == 3)),
                         r=[cdiag, rw], w=[pc])
                yield
                hh = cc % 4
                if cc < 8:
                    q_ = qs[0]
                    sq_ = sqbB[cc % 2]
                    t_ = tmpB[cc % 2]
                    dstT = qT if cc < 4 else kT
                    e_ = eB[cc % 2]
                    act_sigmoid(e_[:, 0:BW], e_, pc[:, 0:BW], pc)
                    S.op("dve", lambda: V.tensor_tensor(q_[:, 0:BW], pc[:, 0:BW], e_[:, 0:BW], ALU.mult), r=[pc, e_], w=[q_])
                    S.op("pool", lambda: P.tensor_tensor(sq_[:, 0:BW], q_[:, 0:BW], q_[:, 0:BW], ALU.mult), r=[q_], w=[sq_])
                    pn = ps_blk()
                    S.op("pe", lambda: PE.matmul(pn[:, 0:BW], onesb, sq_[:, 0:BW], start=True, stop=True), r=[cb, sq_], w=[pn])
                    yield
                    S.op("act", lambda: A.activation(out=t_[:, 0:BW], in_=pn[:, 0:BW], func=AF.Ln, bias=EPS_RMS, scale=1.0), r=[pn, small], w=[t_])
                    if cc < 4:
                        S.op("act", lambda: A.activation(out=t_[:, 0:BW], in_=t_[:, 0:BW], func=AF.Exp, bias=LNQS, scale=-0.5), r=[t_, small], w=[t_])
                    else:
                        S.op("act", lambda: A.activation(out=t_[:, 0:BW], in_=t_[:, 0:BW], func=AF.Exp, scale=-0.5), r=[t_], w=[t_])
                    yield
                    S.op("dve", lambda: V.tensor_tensor(dstT[:, hh, :], q_[:, 0:BW], t_[:, 0:BW], ALU.mult), r=[q_, t_], w=[dstT])
                else:
                    e_ = eB[cc % 2]
                    act_sigmoid(e_[:, 0:BW], e_, pc[:, 0:BW], pc)
                    S.op("dve", lambda: V.tensor_tensor(vT[:, hh, :], pc[:, 0:BW], e_[:, 0:BW], ALU.mult), r=[pc, e_], w=[vT])
                yield
            for zc in range(4):
                pb = proj_chunk(C_Z + zc * 128)
                yield
                t_ = tmpB[zc % 2]
                act_sigmoid(t_[:, 0:BW], t_, pb[:, 0:BW], pb)
                S.op("dve", lambda: V.scalar_tensor_tensor(ZG[:, zc, :], t_[:, 0:BW], colp[:, 0:1], pb[:, 0:BW], ALU.mult, ALU.mult), r=[t_, colp, pb], w=[ZG])
                yield
            for uc in range(4):
                pb = proj_chunk(C_U + uc * 128)
                yield
                e_ = eB[uc % 2]
                gelu_tanh(UG[:, uc, :], UG, pb[:, 0:BW], pb, e_[:, 0:BW], e_)
                yield
            if blk == 0:
                dump("qT", qT, qT[:], [128, 4, BW], BF16)
                dump("kT", kT, kT[:], [128, 4, BW], BF16)
                dump("vT", vT, vT[:], [128, 4, BW], BF16)

            stage(4)
            yield
            for tl in range(TPB):
                pb = ps_blk()
                for kc in range(8):
                    S.op("pe", lambda: PE.matmul(pb[:], xTb[:, kc, tl * 128:(tl + 1) * 128], w_in_b[:, kc, C_VS:C_VS + 512],
                                                 start=(kc == 0), stop=(kc == 7)), r=[xTb, w_in_b], w=[pb])
                yield
                e_ = eB[tl % 2]
                VG = Buf(e_.t[:].rearrange("p (h j) -> p h j", h=4), e_.k)
                gelu_tanh(VG[:], VG, v3(pb), pb, VG[:], VG)
                S.op("dve", lambda: V.tensor_reduce(st4B[:, 0:4], VG[:], AX.X, ALU.add), r=[VG], w=[st4B])
                S.op("dve", lambda: V.tensor_scalar(st4B[:, 4:8], st4B[:, 0:4], -1.0 / 128, None, ALU.mult), r=[st4B], w=[st4B])
                S.op("dve", lambda: V.tensor_tensor(XC[:], VG[:], bc_j(st4B[:, 4:8]), ALU.add), r=[VG, st4B], w=[XC])
                yield
                S.op("pool", lambda: P.tensor_tensor(VG[:], XC[:], XC[:], ALU.mult), r=[XC], w=[VG])
                S.op("dve", lambda: V.tensor_reduce(st4B[:, 8:12], VG[:], AX.X, ALU.add), r=[VG], w=[st4B])
                yield
                S.op("act", lambda: A.activation(out=st4B[:, 8:12], in_=st4B[:, 8:12], func=AF.Ln, bias=EPS_LN, scale=1.0 / 128), r=[st4B, small], w=[st4B])
                S.op("act", lambda: A.activation(out=st4B[:, 12:16], in_=st4B[:, 8:12], func=AF.Exp, scale=-0.5), r=[st4B], w=[st4B])
                S.op("dve", lambda: V.tensor_tensor(XC[:], XC[:], bc_j(st4B[:, 12:16]), ALU.mult), r=[XC, st4B], w=[XC])
                yield
                S.op("pool", lambda: P.tensor_tensor(XC[:], XC[:], R("sgu_g").rearrange("p (g d) -> p g d", g=4), ALU.mult), r=[XC, rowp], w=[XC])
                S.op("dve", lambda: V.tensor_tensor(VLN[bs * TPB + tl][:].rearrange("p (g d) -> p g d", g=4), XC[:], R("sgu_b").rearrange("p (g d) -> p g d", g=4), ALU.add),
                     r=[XC, rowp], w=[VLN[bs * TPB + tl]])
                yield

            if blk == 0:
                dump("VLN0", VLN[bs * TPB], VLN[bs * TPB][:], [128, 512], BF16)
        def tile_gen(blk, tl):
            if True:
                stage(5)
                bs = blk % 2
                qT, kT, vT, ZG, UG = qT2[bs], kT2[bs], vT2[bs], ZG2[bs], UG2[bs]
                ti = blk * TPB + tl
                par = ti % 2
                cf, EG, AinT, QDT, KD, Uf, WTb = cf2[par], EG2[par], AinT2[par], QDT2[par], KD2[par], Uf2[par], WTb2[par]
                tc0 = tl * 128
                tcs = slice(tc0, tc0 + 128)
                s8 = sp8[bs * TPB + tl]
                cfk = [cf]
                S.op("act", lambda: A.activation(out=cf[:, 0:4], in_=s8[:, 0:4], func=AF.Exp, scale=-1.0), r=[s8], w=cfk)
                S.op("act", lambda: A.activation(out=cf[:, 0:4], in_=cf[:, 0:4], func=AF.Ln, bias=ONE, scale=1.0), r=[cf, small], w=cfk)
                S.op("dve", lambda: V.tensor_scalar(cf[:, 0:4], cf[:, 0:4], -1.0, None, ALU.mult), r=cfk, w=cfk)
                yield
                S.op("act", lambda: A.activation(out=cf[:, 4:8], in_=cf[:, 0:4], func=AF.Exp), r=cfk, w=cfk)
                S.op("dve", lambda: V.tensor_tensor(cf[:, 8:12], s8[:, 4:8], R("dt_bias"), ALU.add), r=[s8, rowp], w=cfk)
                S.op("act", lambda: A.activation(out=cf[:, 8:12], in_=cf[:, 8:12], func=AF.Exp), r=cfk, w=cfk)
                yield
                S.op("act", lambda: A.activation(out=cf[:, 8:12], in_=cf[:, 8:12], func=AF.Ln, bias=ONE, scale=1.0), r=[cf, small], w=cfk)
                S.op("dve", lambda: V.tensor_tensor(cf[:, 8:12], cf[:, 8:12], small[:, 0:4], ALU.mult), r=[cf, small], w=cfk)
                pb = ps_tile()
                S.op("pe", lambda: PE.matmul(pb[:, 0:4], C("tri"), cf[:, 8:12], start=True, stop=True), r=[cst, cf], w=[pb])
                S.op("pe", lambda: PE.matmul(pb[:, 4:8], C("chs"), cf[:, 8:12], start=True, stop=True), r=[cst, cf], w=[pb])
                yield
                S.op("dve", lambda: V.tensor_copy(cf[:, 12:20], pb[:, 0:8]), r=[pb], w=cfk)
                S.op("dve", lambda: V.tensor_tensor(cf[:, 20:24], cf[:, 12:16], cf[:, 0:4], ALU.add), r=cfk, w=cfk)
                S.op("dve", lambda: V.tensor_scalar(cf[:, 24:28], cf[:, 12:16], -1.0, None, ALU.mult), r=cfk, w=cfk)
                yield
                S.op("act", lambda: A.activation(out=cf[:, 28:32], in_=cf[:, 12:16], func=AF.Exp), r=cfk, w=cfk)
                S.op("dve", lambda: V.tensor_tensor(cf[:, 32:36], cf[:, 4:8], cf[:, 28:32], ALU.mult), r=cfk, w=cfk)
                S.op("dve", lambda: V.tensor_tensor(cf[:, 36:40], cf[:, 16:20], cf[:, 12:16], ALU.subtract), r=cfk, w=cfk)
                yield
                S.op("act", lambda: A.activation(out=cf[:, 36:40], in_=cf[:, 36:40], func=AF.Exp), r=cfk, w=cfk)
                S.op("dve", lambda: V.tensor_scalar(cf[:, 40:44], cf[:, 36:40], CM[0], None, ALU.mult), r=[cf, small], w=cfk)
                S.op("dve", lambda: V.tensor_scalar(cf[:, 44:48], cf[:, 36:40], CM[1], None, ALU.mult), r=[cf, small], w=cfk)
                GC, A_, NGC, BETA, CW = cf[:, 12:16], cf[:, 20:24], cf[:, 24:28], cf[:, 4:8], cf[:, 32:36]
                if ti == 0:
                    dump("cf", cf, cf[:], [128, 64])
                stage(6)
                yield
                S.op("dve", lambda: V.tensor_tensor(Dg[:], bc_h(identf), bc_j(GC), ALU.mult), r=[cst, cf], w=[Dg])
                pgc = ps_tile()
                S.op("pe", lambda: PE.matmul(pgc[:], onesf, Dg[:].rearrange("p h j -> p (h j)"), start=True, stop=True), r=[cst, Dg], w=[pgc])
                yield
                S.op("dve", lambda: V.scalar_tensor_tensor(X1[:], v3(pgc), -1.0, bc_h(C("maskS")), ALU.mult, ALU.add), r=[pgc, cst], w=[X1])
                S.op("dve", lambda: V.tensor_tensor(X3[:], v3(pgc), bc_h(C("maskIT")), ALU.add), r=[pgc, cst], w=[X3])
                S.op("act", lambda: A.activation(out=EG[:], in_=v3(pgc), func=AF.Exp), r=[pgc], w=[EG])
                for h in range(4):
                    yield
                    S.op("act", lambda: A.activation(out=E1[:, h, :], in_=X1[:, h, :], func=AF.Exp, bias=cf[:, 20 + h:21 + h], scale=1.0), r=[X1, cf], w=[E1])
                    S.op("act", lambda: A.activation(out=E3[:, h, :], in_=X3[:, h, :], func=AF.Exp, bias=cf[:, 24 + h:25 + h], scale=1.0), r=[X3, cf], w=[E3])
                if ti == 0:
                    dump("E1", E1, E1[:], [128, 4, 128])
                    dump("E3", E3, E3[:], [128, 4, 128])
                    dump("EG", EG, EG[:], [128, 4, 128])
                stage(7)
                yield
                pk = ps_tile()
                for h in range(4):
                    S.op("pe", lambda: PE.matmul(pk[:, h * 128:(h + 1) * 128], kT[:, h, tcs], kT[:, h, tcs], start=True, stop=True), r=[kT], w=[pk])
                yield
                S.op("dve", lambda: V.scalar_tensor_tensor(Mb[0][:], v3(pk), -1.0, E1[:], ALU.mult, ALU.mult), r=[pk, E1], w=[Mb[0]])
                ptm = ps_tile()
                ptm_b = ptm[:].bitcast(BF16)
                for h in range(4):
                    S.op("pe", lambda: PE.transpose(ptm_b[:, h * 128:(h + 1) * 128], Mb[0][:, h, :], identb), r=[Mb[0], cb], w=[ptm])
                yield
                S.op("act", lambda: A.activation(out=MTb[0][:], in_=ptm_b[:, 0:512].rearrange("p (h j) -> p h j", h=4), func=AF.Copy), r=[ptm], w=[MTb[0]])
                S.op("pool", lambda: P.tensor_tensor(Pb[0][:], MTb[0][:], bc_h(identb), ALU.add), r=[MTb[0], cb], w=[Pb[0]])
                pq = ps_tile()
                for h in range(4):
                    S.op("pe", lambda: PE.matmul(pq[:, h * 128:(h + 1) * 128], kT[:, h, tcs], qT[:, h, tcs], start=True, stop=True), r=[kT, qT], w=[pq])
                yield
                S.op("dve", lambda: V.tensor_tensor(AinT[:], v3(pq), E3[:], ALU.mult), r=[pq, E3], w=[AinT])
                S.op("pool", lambda: P.tensor_tensor(QDT[:], qT[:, :, tcs], EG[:], ALU.mult), r=[qT, EG], w=[QDT])
                ptk = ps_tile()
                ptv = ps_tile()
                ptk_b = ptk[:].bitcast(BF16)
                ptv_b = ptv[:].bitcast(BF16)
                for h in range(4):
                    S.op("pe", lambda: PE.transpose(ptk_b[:, h * 128:(h + 1) * 128], kT[:, h, tcs], identb), r=[kT, cb], w=[ptk])
                    S.op("pe", lambda: PE.transpose(ptv_b[:, h * 128:(h + 1) * 128], vT[:, h, tcs], identb), r=[vT, cb], w=[ptv])
                k3 = ptk_b[:, 0:512].rearrange("p (h j) -> p h j", h=4)
                vv3 = ptv_b[:, 0:512].rearrange("p (h j) -> p h j", h=4)
                yield
                S.op("dve", lambda: V.tensor_tensor(RHSw[:], k3, bc_j(CW), ALU.mult), r=[ptk, cf], w=[RHSw])
                S.op("dve", lambda: V.tensor_tensor(KD[0][:], k3, bc_j(cf[:, 40:44]), ALU.mult), r=[ptk, cf], w=[KD[0]])
                S.op("dve", lambda: V.tensor_tensor(KD[1][:], k3, bc_j(cf[:, 44:48]), ALU.mult), r=[ptk, cf], w=[KD[1]])
                yield
                S.op("dve", lambda: V.tensor_tensor(RHSu[:], vv3, bc_j(BETA), ALU.mult), r=[ptv, cf], w=[RHSu])
                if ti == 0:
                    dump("M0", Mb[0], Mb[0][:], [128, 4, 128], BF16)
                    dump("MT0", MTb[0], MTb[0][:], [128, 4, 128], BF16)
                    dump("AinT", AinT, AinT[:], [128, 4, 128], BF16)
                    dump("RHSu", RHSu, RHSu[:], [128, 4, 128], BF16)
                    dump("RHSw", RHSw, RHSw[:], [128, 4, 128], BF16)
                stage(8)
                yield
                for s_ in range(6):
                    cur, nxt = s_ % 2, (s_ + 1) % 2
                    if s_ >= 1:
                        pP = ps_tile()
                        for h in range(4):
                            S.op("pe", lambda: PE.matmul(pP[:, h * 128:(h + 1) * 128], Mb[cur][:, h, :], Pb[nxt][:, h, :], start=True, stop=True),
                                 r=[Mb[cur], Pb[nxt]], w=[pP])
                    if s_ < 5:
                        pM = ps_tile()
                        for h in range(4):
                            S.op("pe", lambda: PE.matmul(pM[:, h * 128:(h + 1) * 128], MTb[cur][:, h, :], Mb[cur][:, h, :], start=True, stop=True),
                                 r=[Mb[cur], MTb[cur]], w=[pM])
                    if s_ < 4:
                        pMT = ps_tile()
                        for h in range(4):
                            S.op("pe", lambda: PE.matmul(pMT[:, h * 128:(h + 1) * 128], Mb[cur][:, h, :], MTb[cur][:, h, :], start=True, stop=True),
                                 r=[Mb[cur], MTb[cur]], w=[pMT])
                    if s_ >= 1:
                        yield
                        S.op("dve", lambda: V.tensor_tensor(Pb[cur][:], Pb[nxt][:], v3(pP), ALU.add), r=[Pb[nxt], pP], w=[Pb[cur]])
                    if s_ < 5:
                        S.op("act", lambda: A.activation(out=Mb[nxt][:], in_=v3(pM), func=AF.Copy), r=[pM], w=[Mb[nxt]])
                    if s_ < 4:
                        S.op("dve", lambda: V.tensor_copy(MTb[nxt][:], v3(pMT)), r=[pMT], w=[MTb[nxt]])
                    yield
                stage(9)
                yield
                TT = Pb[1]
                pu = ps_tile()
                pw = ps_tile()
                for h in range(4):
                    S.op("pe", lambda: PE.matmul(pu[:, h * 128:(h + 1) * 128], TT[:, h, :], RHSu[:, h, :], start=True, stop=True), r=[TT, RHSu], w=[pu])
                    S.op("pe", lambda: PE.matmul(pw[:, h * 128:(h + 1) * 128], RHSw[:, h, :], TT[:, h, :], start=True, stop=True), r=[TT, RHSw], w=[pw])
                yield
                S.op("act", lambda: A.activation(out=Uf[:], in_=v3(pu), func=AF.Copy), r=[pu], w=[Uf])
                S.op("act", lambda: A.activation(out=WTb[:], in_=v3(pw), func=AF.Copy), r=[pw], w=[WTb])
                if ti == 0:
                    dump("TT", TT, TT[:], [128, 4, 128], BF16)
                    dump("Uf", Uf, Uf[:], [128, 4, 128])
                    dump("WTb", WTb, WTb[:], [128, 4, 128], BF16)
                po = ps_acc()
                for c in range(2):
                    cs_ = slice(c * 64, c * 64 + 64)
                    pws = ps_tile()
                    for h in range(4):
                        S.op("pe", lambda: PE.matmul(pws[:, h * 128:(h + 1) * 128], WTb[:, h, :], Sb[:, h, :], start=True, stop=True), r=[WTb, Sb], w=[pws])
                    yield
                    S.op("dve", lambda: V.tensor_tensor(VN[:], Uf[:], v3(pws), ALU.subtract), r=[Uf, pws], w=[VN])
                    yield
                    for h in range(4):
                        oc = slice(h * 128 + c * 64, h * 128 + c * 64 + 64)
                        S.op("pe", lambda: PE.matmul(po[:, oc], Sb[:, h, :], QDT[:, h, cs_], start=True, stop=False), r=[Sb, QDT], w=[po])
                        S.op("pe", lambda: PE.matmul(po[:, oc], VN[:, h, :], AinT[:, h, cs_], start=False, stop=True), r=[VN, AinT], w=[po])
                    pS = ps_tile()
                    for h in range(4):
                        S.op("pe", lambda: PE.matmul(pS[:, h * 128:(h + 1) * 128], KD[c][:, h, :], VN[:, h, :], start=True, stop=True), r=[KD[c], VN], w=[pS])
                    for h in range(4):
                        gl = EG[:, h, c * 64 + 63:c * 64 + 64]
                        yield
                        S.op("dve", lambda: V.scalar_tensor_tensor(Sf[:, h, :], Sf[:, h, :], gl, pS[:, h * 128:(h + 1) * 128], ALU.mult, ALU.add),
                             r=[Sf, EG, pS], w=[Sf])
                    S.op("act", lambda: A.activation(out=Sb[:], in_=Sf[:], func=AF.Copy), r=[Sf], w=[Sb])
                    yield
                S.op("act", lambda: A.activation(out=OT[:], in_=v3(po), func=AF.Copy), r=[po], w=[OT])
                if ti == 0:
                    dump("OT0", OT, OT[:], [128, 4, 128])
                if ti == 1:
                    dump("OT1", OT, OT[:], [128, 4, 128])
                stage(10)
                yield "Q"
                sq_ = sqb[0]
                S.op("pool", lambda: P.tensor_tensor(sq_[:], OT[:].rearrange("p h j -> p (h j)"), OT[:].rearrange("p h j -> p (h j)"), ALU.mult), r=[OT], w=[sq_])
                pss = ps_q()
                S.op("pe", lambda: PE.matmul(pss[:], onesb, sq_[:], start=True, stop=True), r=[cb, sq_], w=[pss])
                t_ = tQ1
                yield
                S.op("act", lambda: A.activation(out=t_[:], in_=pss[:], func=AF.Ln, bias=EPS_RMS, scale=1.0 / 128), r=[pss, small], w=[t_])
                S.op("act", lambda: A.activation(out=t_[:], in_=t_[:], func=AF.Exp, scale=-0.5), r=[t_], w=[t_])
                S.op("dve", lambda: V.tensor_tensor(t_[:], t_[:], OT[:].rearrange("p h j -> p (h j)"), ALU.mult), r=[t_, OT], w=[t_])
                yield
                S.op("pool", lambda: P.tensor_tensor(YT[:, 0:4, tcs], t_[:].rearrange("p (h j) -> p h j", h=4), ZG[:, :, tcs], ALU.mult), r=[t_, ZG], w=[YT])
                stage(11)
                yield
                pm = ps_q()
                for g in range(4):
                    S.op("pe", lambda: PE.matmul(pm[:, g * 128:(g + 1) * 128], VLN[bs * TPB + tl][:, g * 128:(g + 1) * 128], wstb[:, g, :], start=True, stop=True),
                         r=[VLN[bs * TPB + tl], wstb], w=[pm])
                t2_ = tQ2
                yield
                S.op("dve", lambda: V.tensor_tensor(t2_[:], pm[:], R("bsp"), ALU.add), r=[pm, rowp], w=[t2_])
                S.op("pool", lambda: P.tensor_tensor(YT[:, 4:8, tcs], t2_[:].rearrange("p (h j) -> p h j", h=4), UG[:, :, tcs], ALU.mult), r=[t2_, UG], w=[YT])
                if ti == 0:
                    dump("YT0", YT, YT[:, :, 0:128], [128, 8, 128], BF16)
                stage(12)
                yield
                xr_ = xr[0]
                S.dma("sp", ("xr", 0), lambda q: q.dma_start(out=xr_[:], in_=x_t[ti]), w=[xr_])
                for half in range(2):
                    px = ps_q()
                    for c8 in range(8):
                        S.op("pe", lambda: PE.matmul(px[:], YT[:, c8, tcs], w_out_b[:, c8, half * 512:(half + 1) * 512], start=(c8 == 0), stop=(c8 == 7)),
                             r=[YT, w_out_b], w=[px])
                    yield
                    S.op("dve", lambda: V.scalar_tensor_tensor(hp[:, half * 512:(half + 1) * 512], xr_[:, half * 512:(half + 1) * 512], ALPHA, px[:], ALU.mult, ALU.add),
                         r=[xr_, px], w=[hp])
                h1_ = h1[0]
                h1b_ = h1b[0]
                yield from ln_rows(hp, h1_, R("ln1_g"), R("ln1_b"), st4)
                S.dma("sp", ("h1st", 0), lambda q: q.dma_start(out=h1_t[ti], in_=h1_[:]), r=[h1_], w=[("h1_d", ti)])
                S.op("pool", lambda: P.tensor_copy(h1b_[:], h1_[:]), r=[h1_], w=[h1b_])
                if ti == 0:
                    dump("h1_0", h1_, h1_[:], [128, D])
                stage(13)
                yield
                for half in range(2):
                    pt = ps_q()
                    for qd in range(4):
                        kc = half * 4 + qd
                        S.op("pe", lambda: PE.transpose(pt[:, qd * 128:(qd + 1) * 128], h1_[:, kc * 128:(kc + 1) * 128], identf), r=[h1_, cst], w=[pt])
                    yield
                    S.op("act", lambda: A.activation(out=h1T[:, half * 4:half * 4 + 4, :], in_=v3(pt), func=AF.Copy), r=[pt], w=[h1T])
                pl = ps_q()
                for kc in range(8):
                    S.op("pe", lambda: PE.matmul(pl[:, 0:72], h1T[:, kc, :], wrt[:, kc, :], start=(kc == 0), stop=(kc == 7)), r=[h1T, wrt], w=[pl])
                rk = [rt]
                LG = rt[:, 0:72]
                yield
                S.op("dve", lambda: V.tensor_tensor(LG, pl[:, 0:72], R("br"), ALU.add), r=[pl, rowp], w=rk)
                S.op("dve", lambda: V.max(rt[:, 72:80], rt[:, 0:8]), r=rk, w=rk)
                S.op("dve", lambda: V.tensor_scalar(rt[:, 80:88], rt[:, 0:8], rt[:, 72:73], None, ALU.is_equal), r=rk, w=rk)
                yield
                S.op("dve", lambda: V.tensor_scalar(rt[:, 98:99], rt[:, 72:73], -1.0, None, ALU.mult), r=rk, w=rk)
                S.op("act", lambda: A.activation(out=rt[:, 88:96], in_=rt[:, 0:8], func=AF.Exp, bias=rt[:, 98:99], scale=1.0), r=rk, w=rk)
                S.op("dve", lambda: V.tensor_reduce(rt[:, 96:97], rt[:, 88:96], AX.X, ALU.add), r=rk, w=rk)
                yield
                S.op("dve", lambda: V.reciprocal(rt[:, 97:98], rt[:, 96:97]), r=rk, w=rk)
                el3 = rt[:, 8:72].rearrange("p (g j) -> p g j", g=8)
                tm3 = rt[:, 100:164].rearrange("p (g j) -> p g j", g=8)
                S.op("dve", lambda: V.tensor_tensor(tm3, el3, rt[:, 80:88].unsqueeze(2).broadcast_to([128, 8, 8]), ALU.mult), r=rk, w=rk)
                S.op("dve", lambda: V.tensor_reduce(rt[:, 164:172], rt[:, 100:164].rearrange("p (g j) -> p j g", g=8), AX.X, ALU.add), r=rk, w=rk)
                yield
                S.op("dve", lambda: V.max(rt[:, 172:180], rt[:, 164:172]), r=rk, w=rk)
                S.op("dve", lambda: V.tensor_scalar(rt[:, 180:188], rt[:, 164:172], rt[:, 172:173], None, ALU.is_equal), r=rk, w=rk)
                S.op("dve", lambda: V.tensor_scalar(rt[:, 188:196], rt[:, 164:172], rt[:, 173:174], None, ALU.is_equal), r=rk, w=rk)
                yield
                S.op("dve", lambda: V.tensor_tensor(rt[:, 196:197], rt[:, 173:174], rt[:, 172:173], ALU.subtract), r=rk, w=rk)
                S.op("act", lambda: A.activation(out=rt[:, 197:198], in_=rt[:, 196:197], func=AF.Exp), r=rk, w=rk)
                S.op("dve", lambda: V.tensor_scalar(rt[:, 198:199], rt[:, 197:198], 1.0, None, ALU.add), r=rk, w=rk)
                yield
                S.op("dve", lambda: V.reciprocal(rt[:, 198:199], rt[:, 198:199]), r=rk, w=rk)
                S.op("dve", lambda: V.tensor_tensor(rt[:, 199:200], rt[:, 197:198], rt[:, 198:199], ALU.mult), r=rk, w=rk)
                S.op("dve", lambda: V.tensor_scalar(RI[:, ti, 2:4], rt[:, 198:200], rt[:, 97:98], None, ALU.mult), r=rk, w=[RI])
                oh0 = rt[:, 200:264].rearrange("p (g j) -> p g j", g=8)
                oh1 = rt[:, 264:328].rearrange("p (g j) -> p g j", g=8)
                gohb = rt[:, 80:88].unsqueeze(2).broadcast_to([128, 8, 8])
                yield
                S.op("dve", lambda: V.tensor_tensor(oh0, gohb, rt[:, 180:188].unsqueeze(1).broadcast_to([128, 8, 8]), ALU.mult), r=rk, w=rk)
                S.op("dve", lambda: V.tensor_tensor(oh1, gohb, rt[:, 188:196].unsqueeze(1).broadcast_to([128, 8, 8]), ALU.mult), r=rk, w=rk)
                S.op("dve", lambda: V.tensor_tensor(OHb[:], rt[:, 200:264], rt[:, 264:328], ALU.add), r=rk, w=[OHb])
                pp = ps_q()
                S.op("pe", lambda: PE.matmul(pp[:, 0:64], sltrib, OHb[:], start=True, stop=True), r=[cb, OHb], w=[pp])
                S.op("pe", lambda: PE.matmul(pp[:, 64:128], onesb, OHb[:], start=True, stop=True), r=[cb, OHb], w=[pp])
                yield
                S.op("dve", lambda: V.tensor_tensor(rt[:, 328:392], pp[:, 0:64], carry[:], ALU.add), r=[pp, carry], w=rk)
                S.op("dve", lambda: V.tensor_tensor(carry[:], carry[:], pp[:, 64:128], ALU.add), r=[pp, carry], w=[carry])
                for k2 in range(2):
                    ohk = rt[:, 200 + 64 * k2:264 + 64 * k2]
                    S.op("dve", lambda: V.tensor_tensor(rt[:, 392:456], rt[:, 328:392], ohk, ALU.mult), r=rk, w=rk)
                    yield
                    S.op("dve", lambda: V.tensor_reduce(rt[:, 456 + k2:457 + k2], rt[:, 392:456], AX.X, ALU.add), r=rk, w=rk)
                    S.op("dve", lambda: V.tensor_tensor(rt[:, 392:456], R("iota"), ohk, ALU.mult), r=[rt, rowp], w=rk)
                    S.op("dve", lambda: V.tensor_reduce(rt[:, 458 + k2:459 + k2], rt[:, 392:456], AX.X, ALU.add), r=rk, w=rk)
                stage(14)
                yield
                S.op("dve", lambda: V.scalar_tensor_tensor(rt[:, 462:464], rt[:, 458:460], float(CAP), rt[:, 456:458], ALU.mult, ALU.add), r=rk, w=rk)
                S.op("dve", lambda: V.tensor_scalar(rt[:, 460:462], rt[:, 456:458], float(CAP), None, ALU.is_ge), r=rk, w=rk)
                S.op("dve", lambda: V.tensor_scalar(rt[:, 464:466], rt[:, 462:464], -1.0, float(TRASH), ALU.mult, ALU.add), r=rk, w=rk)
                yield
                S.op("dve", lambda: V.tensor_tensor(rt[:, 464:466], rt[:, 464:466], rt[:, 460:462], ALU.mult), r=rk, w=rk)
                S.op("dve", lambda: V.tensor_tensor(rt[:, 462:464], rt[:, 462:464], rt[:, 464:466], ALU.add), r=rk, w=rk)
                S.op("dve", lambda: V.tensor_scalar(RI[:, ti, 0:2], rt[:, 462:464], float(TRASH), 0.0, ALU.min, ALU.max), r=rk, w=[RI])
                S.op("dve", lambda: V.tensor_copy(RIi[:, ti, :], RI[:, ti, 0:2]), r=[RI], w=[RIi])
                stage(15)
                yield
                for k2 in range(2):
                    S.dma("pool", ("sc", k2), lambda q, k2=k2: q.indirect_dma_start(
                        out=xs_d, out_offset=bass.IndirectOffsetOnAxis(ap=RIi[:, ti, k2:k2 + 1], axis=0),
                        in_=h1b_[:], in_offset=None),
                        r=[h1b_, RIi] + [("xs_zero_last", i) for i in range(4)], w=[("xs_sc", ti, k2)])
                if ti == 0:
                    dump("rt0", rt, rt[:], [128, 512])

        n_tiles = n_blocks * TPB
        blk_done = [False] * (n_blocks + 2)
        q_done = [False] * (n_tiles + 2 * TPB)
        bgen = block_gen(0)
        b_cur = 0
        for _ in bgen:
            pass
        blk_done[0] = True
        bgen = None
        b_next = 1
        ps_gen, ps_tile_i = None, -1
        next_tile = 0
        q_queue = []
        ot_pending = False
        while True:
            progressed = False
            if ps_gen is None and next_tile < n_tiles and blk_done[next_tile // TPB] and not ot_pending:
                ps_gen, ps_tile_i = tile_gen(next_tile // TPB, next_tile % TPB), next_tile
                next_tile += 1
            if ps_gen is not None:
                progressed = True
                r = next(ps_gen)
                if r == "Q":
                    q_queue.append([ps_tile_i, ps_gen, 0])
                    ps_gen = None
                    ot_pending = True
            if q_queue:
                progressed = True
                ent = q_queue[0]
                try:
                    next(ent[1])
                    ent[2] += 1
                    if ent[2] == 1:
                        ot_pending = False
                except StopIteration:
                    q_done[ent[0]] = True
                    q_queue.pop(0)
                    if ent[2] == 0:
                        ot_pending = False
            if bgen is None and b_next < n_blocks and (b_next < 2 or q_done[(b_next - 2) * TPB + TPB - 1]):
                bgen, b_cur = block_gen(b_next), b_next
                b_next += 1
            if bgen is not None:
                progressed = True
                for _ in range(BLOCK_STEPS_PER_TILE_STEP):
                    try:
                        next(bgen)
                    except StopIteration:
                        blk_done[b_cur] = True
                        bgen = None
                        break
            if not progressed:
                break
        assert next_tile == n_tiles and not q_queue and ps_gen is None

        dump("RI", RI, RI[:], [128, NT, 4])

        S.barrier()
        es1.close()
        if stop_after == 1:
            raise _Cut()

        es2 = es.enter_context(ExitStack())
        NW = 3
        wgs = [sb(es2, "wg%d" % i, [128, 8, 512], BF16) for i in range(NW)]
        wus = [sb(es2, "wu%d" % i, [128, 8, 512], BF16) for i in range(NW)]
        wds = [sb(es2, "wd%d" % i, [128, 4, D], BF16) for i in range(NW)]
        Xe = [sb(es2, "Xe%d" % i, [128, RT, D], BF16) for i in range(NW)]
        xTe = [sb(es2, "xTe%d" % i, [128, 8, CAP], BF16) for i in range(NW)]
        sg = [sb(es2, "sg%d" % i, [128, CAP]) for i in range(2)]
        hid = [sb(es2, "hid%d" % i, [128, 4, CAP], BF16) for i in range(NW)]
        ysb = [sb(es2, "ysb%d" % i, [128, RT, D], BF16) for i in range(NW)]
        xs_e = xs_d[0:NE * CAP, :].rearrange("(e r p) d -> e p r d", p=128, r=RT)
        ys_e = ys_d[0:NE * CAP, :].rearrange("(e r p) d -> e p r d", p=128, r=RT)

        def load_expert(e):
            sl = e % NW
            S.dma("pool", ("wg", sl), lambda q: q.dma_start(out=wgs[sl][:], in_=wg_d[e].rearrange("(kc p) f -> p kc f", p=128)), w=[wgs[sl]])
            S.dma("pool", ("wu", sl), lambda q: q.dma_start(out=wus[sl][:], in_=wu_d[e].rearrange("(kc p) f -> p kc f", p=128)), w=[wus[sl]])
            S.dma("pool", ("wd", sl), lambda q: q.dma_start(out=wds[sl][:], in_=wd_d[e].rearrange("(fc p) d -> p fc d", p=128)), w=[wds[sl]])
            S.dma("sp", ("xe", e % NW), lambda q: q.dma_start(out=Xe[e % NW][:], in_=xs_e[e]), w=[Xe[e % NW]])

        for e in range(min(NW, n_experts)):
            load_expert(e)
        evac_i = [0]

        def evac(dst_ap, src_ap, rbuf, wbuf):
            if evac_i[0] % 2 == 0:
                S.op("act", lambda: A.activation(out=dst_ap, in_=src_ap, func=AF.Copy), r=[rbuf], w=[wbuf])
            else:
                S.op("dve", lambda: V.tensor_copy(dst_ap, src_ap), r=[rbuf], w=[wbuf])
            evac_i[0] += 1

        for e in range(n_experts):
            sl = e % NW
            X_ = Xe[e % NW]
            xT_ = xTe[e % NW]
            hid_ = hid[e % NW]
            ys_ = ysb[e % NW]
            for r_ in range(RT):
                pt = ps_next()
                pt_b = pt[:].bitcast(BF16)
                for kc in range(8):
                    S.op("pe", lambda: PE.transpose(pt_b[:, kc * 128:(kc + 1) * 128], X_[:, r_, kc * 128:(kc + 1) * 128], identb), r=[X_, cb], w=[pt])
                evac(xT_[:, :, r_ * 128:(r_ + 1) * 128], pt_b.rearrange("p (k j) -> p k j", k=8), pt, xT_)
            for fc in range(4):
                pg = ps_next()
                for kc in range(8):
                    S.op("pe", lambda: PE.matmul(pg[:, 0:CAP], wgs[sl][:, kc, fc * 128:(fc + 1) * 128], xT_[:, kc, :], start=(kc == 0), stop=(kc == 7)),
                         r=[wgs[sl], xT_], w=[pg])
                pu_ = ps_next()
                for kc in range(8):
                    S.op("pe", lambda: PE.matmul(pu_[:, 0:CAP], wus[sl][:, kc, fc * 128:(fc + 1) * 128], xT_[:, kc, :], start=(kc == 0), stop=(kc == 7)),
                         r=[wus[sl], xT_], w=[pu_])
                sg_ = sg[fc % 2]
                S.op("act", lambda: A.activation(out=sg_[:], in_=pg[:, 0:CAP], func=AF.Silu), r=[pg], w=[sg_])
                S.op("dve", lambda: V.tensor_tensor(hid_[:, fc, :], sg_[:], pu_[:, 0:CAP], ALU.mult), r=[sg_, pu_], w=[hid_])
            for r_ in range(RT):
                for dh in range(2):
                    py = ps_next()
                    for fc in range(4):
                        S.op("pe", lambda: PE.matmul(py[:], hid_[:, fc, r_ * 128:(r_ + 1) * 128], wds[sl][:, fc, dh * 512:(dh + 1) * 512], start=(fc == 0), stop=(fc == 3)),
                             r=[hid_, wds[sl]], w=[py])
                    evac(ys_[:, r_, dh * 512:(dh + 1) * 512], py[:], py, ys_)
            S.dma("sp", ("yst", e % NW), lambda q: q.dma_start(out=ys_e[e], in_=ys_[:]), r=[ys_], w=[("ys_e", e)])
            if e + NW < n_experts:
                load_expert(e + NW)
        S.barrier()
        es2.close()
        if stop_after == 2:
            raise _Cut()

        es3 = es.enter_context(ExitStack())
        NS3 = 6
        y0 = [sb(es3, "y0_%d" % i, [128, D], BF16) for i in range(NS3)]
        y1 = [sb(es3, "y1_%d" % i, [128, D], BF16) for i in range(NS3)]
        h1r = [sb(es3, "h1r%d" % i, [128, D]) for i in range(NS3)]
        accs = [sb(es3, "acc%d" % i, [128, D]) for i in range(NS3)]
        ob = [sb(es3, "ob%d" % i, [128, D]) for i in range(NS3)]
        junk = sb(es3, "junk3", [128, D])
        st3s = [sb(es3, "st3_%d" % i, [128, 16]) for i in range(NS3)]
        rowp2 = sb(es3, "rowp2", [128, 2 * D])
        S.dma("sp", "ld_rowp2", lambda q: q.dma_start(out=rowp2[:], in_=rowp2_d), w=[rowp2])
        for ti in range(NT):
            a0, a1, hr, o_ = y0[ti % NS3], y1[ti % NS3], h1r[ti % NS3], ob[ti % NS3]
            acc, st3 = accs[ti % NS3], st3s[ti % NS3]
            S.dma("pool", ("g0", ti % NS3), lambda q: q.indirect_dma_start(
                out=a0[:], out_offset=None, in_=ys_d, in_offset=bass.IndirectOffsetOnAxis(ap=RIi[:, ti, 0:1], axis=0)), r=[RIi], w=[a0])
            S.dma("pool", ("g1", ti % NS3), lambda q: q.indirect_dma_start(
                out=a1[:], out_offset=None, in_=ys_d, in_offset=bass.IndirectOffsetOnAxis(ap=RIi[:, ti, 1:2], axis=0)), r=[RIi], w=[a1])
            S.dma("sp", ("h1r", ti % NS3), lambda q: q.dma_start(out=hr[:], in_=h1_t[ti]), w=[hr])
            S.op("act", lambda: A.activation(out=acc[:], in_=a0[:], func=AF.Identity, scale=RI[:, ti, 2:3]), r=[a0, RI], w=[acc])
            S.op("dve", lambda: V.scalar_tensor_tensor(acc[:], a1[:], RI[:, ti, 3:4], acc[:], ALU.mult, ALU.add), r=[a1, RI, acc], w=[acc])
            S.op("dve", lambda: V.scalar_tensor_tensor(acc[:], hr[:], ALPHA, acc[:], ALU.mult, ALU.add), r=[hr, acc], w=[acc])
            for _ in ln_rows(acc, o_, rowp2[:, 0:D], rowp2[:, D:2 * D], st3, gbuf=rowp2):
                pass
            S.dma("sp", ("ost", ti % NS3), lambda q: q.dma_start(out=out_t[ti], in_=o_[:]), r=[o_], w=[("out", ti)])
        raise _Cut()


def host_inputs(inputs, b):
    f = np.float32
    g = lambda n: np.asarray(inputs[n], dtype=f)[0]
    rowp = np.zeros((NRP,), f)

    def put(n, v):
        o, w = RP[n]
        rowp[o:o + w] = np.asarray(v, f).reshape(-1)
    put("a_log", g("a_log")); put("dt_bias", g("dt_bias"))
    put("sgu_g", g("sgu_ln_g")); put("sgu_b", g("sgu_ln_b"))
    put("bsp", g("b_spatial"))
    put("ln1_g", g("ln1_g")); put("ln1_b", g("ln1_b"))
    rowp2 = np.ascontiguousarray(np.broadcast_to(np.concatenate([g("ln2_g"), g("ln2_b")])[None, :], (128, 2 * D)))
    put("br", np.concatenate([g("b_router_group"), g("b_router_expert")]))
    put("iota", np.arange(64))
    rowp = np.ascontiguousarray(np.broadcast_to(rowp[None, :], (128, NRP)))
    colp = np.zeros((128, 4), f)
    colp[:, 0] = g("dn_norm_w")
    convw_t = np.ascontiguousarray(g("conv_w").T.reshape(12, 128, 4).transpose(1, 0, 2))
    wst = np.ascontiguousarray(g("w_spatial").transpose(2, 0, 1))
    wr = np.ascontiguousarray(np.concatenate([g("w_router_group"), g("w_router_expert")], axis=1))
    i = np.arange(128)
    same = (i[:, None] // 64) == (i[None, :] // 64)
    cst = np.zeros((128, NCS, 128), f)
    cst[:, CS["ident"], :] = np.eye(128)
    cst[:, CS["ones"], :] = 1.0
    cst[:, CS["tri"], :] = ((i[:, None] <= i[None, :]) & same)
    cst[:, CS["chs"], :] = same
    cst[:, CS["maskS"], :] = np.where((i[None, :] < i[:, None]) & same, 0.0, NEG)
    cst[:, CS["maskST"], :] = np.where((i[None, :] > i[:, None]) & same, 0.0, NEG)
    cst[:, CS["maskIT"], :] = np.where((i[None, :] >= i[:, None]) & same, 0.0, NEG)
    cst[:, CS["sltri"], :] = (i[:, None] < i[None, :])
    cst[:, CS["maskWS"], :] = (i[None, :] >= i[:, None])
    return {
        "x": np.ascontiguousarray(np.asarray(inputs["x"], f)[b]),
        "w_in": g("w_in"), "w_out": g("w_out"), "convw_t": convw_t, "rowp": rowp, "rowp2": rowp2, "colp": colp,
        "wst": wst, "wr": wr, "w_gate": g("w_gate"), "w_up": g("w_up"), "w_down": g("w_down"),
        "cst": cst,
    }


def kernel(**inputs):
    nc, _ = build_program()
    shared = host_inputs(inputs, 0)
    in_maps = []
    for b in range(8):
        m = dict(shared)
        m["x"] = np.ascontiguousarray(np.asarray(inputs["x"], np.float32)[b])
        in_maps.append(m)
    res = run_bass_kernel_spmd(nc, in_maps, core_ids=list(range(8)))
    return np.stack([np.asarray(r["out"], np.float32) for r in res.results], axis=0)
```
